# Optimizing a Trainium2 kernel written in Bass

```python
import math
import jax, jax.numpy as jnp
from jax import lax
import numpy as np

D_MODEL = 1024
BATCH = 4
SEQ = 4096
DEPTH = 1

CHUNK = 64
Q_BLOCK = 128
N_MEM = 256
EPS = 1e-6
NEG_INF = -1e30

D_MIX = D_MODEL
D_SSM = D_MIX // 2
SSM_GROUP = 16
SSM_GROUPS = D_SSM // SSM_GROUP
SSM_STATE = 64
DT_MIN = 1e-3
DT_MAX = 1e-1
D_MLA = D_MIX - D_SSM
MLA_HEADS = 8
MLA_NOPE = 64
MLA_ROPE = 32
MLA_QK = MLA_NOPE + MLA_ROPE
MLA_V = D_MLA // MLA_HEADS
MLA_Q_RANK = 768
MLA_KV_RANK = 256
ROPE_THETA = 10000.0
D_IN = D_SSM + MLA_Q_RANK + MLA_KV_RANK + MLA_ROPE
XATTN_HEADS = 4
XATTN_HEAD_DIM = D_MODEL // XATTN_HEADS
MOE_GROUPS = 4
MOE_PER_GROUP = 8
MOE_EXPERTS = MOE_GROUPS * MOE_PER_GROUP
MOE_TOPK = 2
MOE_FF = D_MODEL // 4

kernel_name = 'hybrid_s5_mla_hmoe_encoder'


def _rms(x, g):
    xf = x.astype(jnp.float32)
    y = xf * lax.rsqrt(jnp.mean(xf * xf, axis=-1, keepdims=True) + EPS)
    return (y * g.astype(jnp.float32)).astype(x.dtype)


def _rope(x, pos):
    half = MLA_ROPE // 2
    inv_freq = ROPE_THETA ** (-jnp.arange(half, dtype=jnp.float32) / half)
    ang = pos.astype(jnp.float32)[..., None] * inv_freq
    if x.ndim == 4:
        ang = ang[:, :, None, :]
    cos, sin = jnp.cos(ang), jnp.sin(ang)
    xf = x.astype(jnp.float32)
    x1, x2 = xf[..., :half], xf[..., half:]
    return jnp.concatenate([x1 * cos - x2 * sin, x1 * sin + x2 * cos], axis=-1).astype(x.dtype)


def _s5_mixer(u, lam_re, lam_im, log_dt, b_re, b_im, c_re, c_im, d_skip, w_glu, b_glu):
    bsz, seq, _ = u.shape
    uf = u.astype(jnp.float32).reshape(bsz, seq, SSM_GROUPS, SSM_GROUP)
    lam = lax.complex(jnp.minimum(lam_re.astype(jnp.float32), -1e-4), lam_im.astype(jnp.float32))
    dt = jnp.exp(log_dt.astype(jnp.float32))[:, None]
    a_bar = jnp.exp(lam * dt)
    b = lax.complex(b_re.astype(jnp.float32), b_im.astype(jnp.float32))
    b_bar = ((a_bar - 1.0) / lam)[..., None] * b
    c = lax.complex(c_re.astype(jnp.float32), c_im.astype(jnp.float32))
    bu = jnp.einsum('gph,bsgh->bsgp', b_bar, uf.astype(jnp.complex64))
    a_full = jnp.broadcast_to(a_bar, bu.shape)

    def combine(earlier, later):
        a_e, s_e = earlier
        a_l, s_l = later
        return a_l * a_e, a_l * s_e + s_l

    _, states = lax.associative_scan(combine, (a_full, bu), axis=1)
    y = jnp.einsum('ghp,bsgp->bsgh', c, states).real
    y = y + d_skip.astype(jnp.float32).reshape(SSM_GROUPS, SSM_GROUP) * uf
    y = jax.nn.gelu(y.reshape(bsz, seq, D_SSM))
    y = y * jax.nn.sigmoid(y @ w_glu.astype(jnp.float32) + b_glu.astype(jnp.float32))
    return y.astype(u.dtype)


def _mla_mixer(c_q, c_kv, k_rope, pos, q_norm_g, w_q_up, kv_norm_g, w_kv_up):
    bsz, seq, _ = c_q.shape
    q = (_rms(c_q, q_norm_g) @ w_q_up).reshape(bsz, seq, MLA_HEADS, MLA_QK)
    q = jnp.concatenate([q[..., :MLA_NOPE], _rope(q[..., MLA_NOPE:], pos)], axis=-1)
    kv = (_rms(c_kv, kv_norm_g) @ w_kv_up).reshape(bsz, seq, MLA_HEADS, MLA_NOPE + MLA_V)
    k_pe = _rope(k_rope, pos)
    k = jnp.concatenate(
        [kv[..., :MLA_NOPE], jnp.broadcast_to(k_pe[:, :, None, :], (bsz, seq, MLA_HEADS, MLA_ROPE))],
        axis=-1)
    v = kv[..., MLA_NOPE:]
    scale = MLA_QK ** -0.5
    n_blk = seq // Q_BLOCK
    q_blocks = q.reshape(bsz, n_blk, Q_BLOCK, MLA_HEADS, MLA_QK).transpose(1, 0, 2, 3, 4)
    key_chunk = jnp.arange(seq) // CHUNK

    def attend(args):
        blk, qb = args
        s = jnp.einsum('bqhd,bkhd->bhqk', qb, k).astype(jnp.float32) * scale
        q_chunk = (blk * Q_BLOCK + jnp.arange(Q_BLOCK)) // CHUNK
        mask = key_chunk[None, :] <= q_chunk[:, None]
        s = jnp.where(mask[None, None], s, NEG_INF)
        p = jax.nn.softmax(s, axis=-1).astype(v.dtype)
        return jnp.einsum('bhqk,bkhd->bqhd', p, v)

    o = lax.map(attend, (jnp.arange(n_blk), q_blocks))
    return o.transpose(1, 0, 2, 3, 4).reshape(bsz, seq, D_MLA)


def _memory_xattn(h, mem_n, w_q, w_k, w_v, w_o):
    bsz, seq, _ = h.shape
    n_mem = mem_n.shape[1]
    q = (h @ w_q).reshape(bsz, seq, XATTN_HEADS, XATTN_HEAD_DIM)
    k = (mem_n @ w_k).reshape(bsz, n_mem, XATTN_HEADS, XATTN_HEAD_DIM)
    v = (mem_n @ w_v).reshape(bsz, n_mem, XATTN_HEADS, XATTN_HEAD_DIM)
    s = jnp.einsum('bshd,bmhd->bhsm', q, k).astype(jnp.float32) * (XATTN_HEAD_DIM ** -0.5)
    p = jax.nn.softmax(s, axis=-1).astype(v.dtype)
    o = jnp.einsum('bhsm,bmhd->bshd', p, v).reshape(bsz, seq, D_MODEL)
    return o @ w_o


def _hier_moe(h, w_group, b_group, w_expert, b_expert, w_gate, w_up, w_down):
    bsz, seq, d = h.shape
    t = h.reshape(bsz * seq, d)
    g_prob = jax.nn.softmax((t @ w_group).astype(jnp.float32) + b_group.astype(jnp.float32), axis=-1)
    g_idx = jnp.argmax(g_prob, axis=-1)
    g_w = jnp.max(g_prob, axis=-1)
    g_onehot = jax.nn.one_hot(g_idx, MOE_GROUPS, dtype=jnp.float32)
    e_logits = jnp.einsum('td,dge->tge', t, w_expert).astype(jnp.float32) + b_expert.astype(jnp.float32)
    e_logits = jnp.sum(e_logits * g_onehot[:, :, None], axis=1)
    top_v, top_i = lax.top_k(e_logits, MOE_TOPK)
    top_w = jax.nn.softmax(top_v, axis=-1) * g_w[:, None]
    eid = g_idx[:, None] * MOE_PER_GROUP + top_i
    gates = jnp.sum(jax.nn.one_hot(eid, MOE_EXPERTS, dtype=jnp.float32) * top_w[..., None], axis=1)
    gates = gates.astype(t.dtype)
    out = jnp.zeros_like(t)
    for g in range(MOE_GROUPS):
        sl = slice(g * MOE_PER_GROUP, (g + 1) * MOE_PER_GROUP)
        a = jax.nn.silu(jnp.einsum('td,edf->tef', t, w_gate[sl])) * jnp.einsum('td,edf->tef', t, w_up[sl])
        a = a * gates[:, sl, None]
        out = out + jnp.einsum('tef,efd->td', a, w_down[sl])
    return out.reshape(bsz, seq, d)


def setup_inputs(seed: int = 0) -> dict:
    key = jax.random.key(seed)
    split = jax.random.split(key, 48)
    ks = iter([split[i] for i in range(48)])
    f32 = jnp.float32
    L = DEPTH

    def nrm(shape, scale):
        return jax.random.normal(next(ks), shape, f32) * scale

    def gain(n):
        return 1.0 + nrm((L, n), 0.02)

    x = nrm((BATCH, SEQ, D_MODEL), 1.0)
    mem = nrm((BATCH, N_MEM, D_MODEL), 1.0)
    offset = jax.random.randint(next(ks), (BATCH, 1), 0, 4096, dtype=jnp.int32)
    positions = offset + jnp.arange(SEQ, dtype=jnp.int32)[None, :]
    n_idx = jnp.arange(SSM_STATE, dtype=f32)
    return {
        'x': x,
        'mem': mem,
        'positions': positions,
        'norm_mix_g': gain(D_MODEL),
        'w_in': nrm((L, D_MODEL, D_IN), D_MODEL ** -0.5),
        'ssm_lam_re': -0.5 + nrm((L, SSM_GROUPS, SSM_STATE), 0.01),
        'ssm_lam_im': math.pi * n_idx + nrm((L, SSM_GROUPS, SSM_STATE), 0.01),
        'ssm_log_dt': jax.random.uniform(next(ks), (L, SSM_GROUPS), f32, math.log(DT_MIN), math.log(DT_MAX)),
        'ssm_b_re': nrm((L, SSM_GROUPS, SSM_STATE, SSM_GROUP), (2 * SSM_GROUP) ** -0.5),
        'ssm_b_im': nrm((L, SSM_GROUPS, SSM_STATE, SSM_GROUP), (2 * SSM_GROUP) ** -0.5),
        'ssm_c_re': nrm((L, SSM_GROUPS, SSM_GROUP, SSM_STATE), (2 * SSM_STATE) ** -0.5),
        'ssm_c_im': nrm((L, SSM_GROUPS, SSM_GROUP, SSM_STATE), (2 * SSM_STATE) ** -0.5),
        'ssm_d': nrm((L, D_SSM), 0.5),
        'ssm_w_glu': nrm((L, D_SSM, D_SSM), D_SSM ** -0.5),
        'ssm_b_glu': nrm((L, D_SSM), 0.01),
        'mla_q_norm_g': gain(MLA_Q_RANK),
        'mla_w_q_up': nrm((L, MLA_Q_RANK, MLA_HEADS * MLA_QK), MLA_Q_RANK ** -0.5),
        'mla_kv_norm_g': gain(MLA_KV_RANK),
        'mla_w_kv_up': nrm((L, MLA_KV_RANK, MLA_HEADS * (MLA_NOPE + MLA_V)), MLA_KV_RANK ** -0.5),
        'out_norm_ssm_g': gain(D_SSM),
        'out_norm_mla_g': gain(D_MLA),
        'w_out': nrm((L, D_MIX, D_MODEL), D_MIX ** -0.5),
        'norm_xattn_g': gain(D_MODEL),
        'norm_mem_g': gain(D_MODEL),
        'xattn_w_q': nrm((L, D_MODEL, D_MODEL), D_MODEL ** -0.5),
        'xattn_w_k': nrm((L, D_MODEL, D_MODEL), D_MODEL ** -0.5),
        'xattn_w_v': nrm((L, D_MODEL, D_MODEL), D_MODEL ** -0.5),
        'xattn_w_o': nrm((L, D_MODEL, D_MODEL), D_MODEL ** -0.5),
        'norm_moe_g': gain(D_MODEL),
        'moe_w_group': nrm((L, D_MODEL, MOE_GROUPS), D_MODEL ** -0.5),
        'moe_b_group': nrm((L, MOE_GROUPS), 0.01),
        'moe_w_expert': nrm((L, D_MODEL, MOE_GROUPS, MOE_PER_GROUP), D_MODEL ** -0.5),
        'moe_b_expert': nrm((L, MOE_GROUPS, MOE_PER_GROUP), 0.01),
        'moe_w_gate': nrm((L, MOE_EXPERTS, D_MODEL, MOE_FF), D_MODEL ** -0.5),
        'moe_w_up': nrm((L, MOE_EXPERTS, D_MODEL, MOE_FF), D_MODEL ** -0.5),
        'moe_w_down': nrm((L, MOE_EXPERTS, MOE_FF, D_MODEL), MOE_FF ** -0.5),
        'norm_final_g': 1.0 + nrm((D_MODEL,), 0.02),
    }


def reference(x, mem, positions, norm_mix_g, w_in, ssm_lam_re, ssm_lam_im, ssm_log_dt,
              ssm_b_re, ssm_b_im, ssm_c_re, ssm_c_im, ssm_d, ssm_w_glu, ssm_b_glu,
              mla_q_norm_g, mla_w_q_up, mla_kv_norm_g, mla_w_kv_up,
              out_norm_ssm_g, out_norm_mla_g, w_out,
              norm_xattn_g, norm_mem_g, xattn_w_q, xattn_w_k, xattn_w_v, xattn_w_o,
              norm_moe_g, moe_w_group, moe_b_group, moe_w_expert, moe_b_expert,
              moe_w_gate, moe_w_up, moe_w_down, norm_final_g):
    s1 = D_SSM
    s2 = s1 + MLA_Q_RANK
    s3 = s2 + MLA_KV_RANK
    for l in range(DEPTH):
        h = _rms(x, norm_mix_g[l])
        z = h @ w_in[l]
        u_ssm, c_q, c_kv, k_rope = z[..., :s1], z[..., s1:s2], z[..., s2:s3], z[..., s3:]
        y_ssm = _s5_mixer(u_ssm, ssm_lam_re[l], ssm_lam_im[l], ssm_log_dt[l], ssm_b_re[l], ssm_b_im[l],
                          ssm_c_re[l], ssm_c_im[l], ssm_d[l], ssm_w_glu[l], ssm_b_glu[l])
        y_mla = _mla_mixer(c_q, c_kv, k_rope, positions, mla_q_norm_g[l], mla_w_q_up[l],
                           mla_kv_norm_g[l], mla_w_kv_up[l])
        y = jnp.concatenate([_rms(y_ssm, out_norm_ssm_g[l]), _rms(y_mla, out_norm_mla_g[l])], axis=-1)
        x = x + y @ w_out[l]
        h = _rms(x, norm_xattn_g[l])
        x = x + _memory_xattn(h, _rms(mem, norm_mem_g[l]), xattn_w_q[l], xattn_w_k[l],
                              xattn_w_v[l], xattn_w_o[l])
        h = _rms(x, norm_moe_g[l])
        x = x + _hier_moe(h, moe_w_group[l], moe_b_group[l], moe_w_expert[l], moe_b_expert[l],
                          moe_w_gate[l], moe_w_up[l], moe_w_down[l])
    return _rms(x, norm_final_g)
```

```python
import math
from contextlib import ExitStack

import numpy as np
import concourse.bass as bass
import concourse.mybir as mybir
from concourse.bass_utils import run_bass_kernel_spmd

F32 = mybir.dt.float32
BF16 = mybir.dt.bfloat16
I32 = mybir.dt.int32
AF = mybir.ActivationFunctionType
ALU = mybir.AluOpType
AX = mybir.AxisListType

D = 1024
T_OWN = 2048
T_ALL = 4096
EPS = 1e-6
MAGIC = 12582912.0
TWO_PI = 2.0 * math.pi
SIN_SCALE = TWO_PI * (1.0 - 2e-6)

NDMA = 32
NDMA_HW = 16


class Buf:
    __slots__ = ("name", "w", "r")

    def __init__(self, name):
        self.name = name
        self.w = []
        self.r = {}


class Sync:
    def __init__(self, nc, stack):
        self.nc = nc
        self.h = {"pe": nc.tensor, "act": nc.scalar, "dve": nc.vector, "pool": nc.gpsimd, "sp": nc.sync}
        self.sem = {k: stack.enter_context(nc.semaphore("s_" + k)) for k in self.h}
        self.cnt = {k: 0 for k in self.h}
        self.seen = {k: {} for k in self.h}
        self.dsem = [stack.enter_context(nc.semaphore("d%d" % i)) for i in range(NDMA)]
        self.dcnt = [0] * NDMA
        self.drr = 0
        self.drr_sw = 0

    def _wait(self, eng, tok):
        kind, key, val = tok
        if kind == "e" and key == eng and eng in ("pe", "sp"):
            return
        k = (kind, key)
        if self.seen[eng].get(k, 0) >= val:
            return
        self.seen[eng][k] = val
        sem = self.sem[key] if kind == "e" else self.dsem[key]
        self.h[eng].wait_ge(sem, val)

    def _deps(self, eng, reads, writes, is_dma=False):
        toks = []
        for b in reads:
            toks.extend(b.w)
        for b in writes:
            for t in b.w:
                if not (is_dma and t[0] == "d"):
                    toks.append(t)
            toks.extend(b.r.values())
        for t in toks:
            self._wait(eng, t)

    def _mark(self, tok, reads, writes):
        for b in reads:
            b.r[(tok[0], tok[1])] = tok
        for b in writes:
            if tok[0] == "d":
                b.w = [t for t in b.w if t[0] == "d"] + [tok]
            else:
                b.w = [tok]
            b.r = {}

    def op(self, eng, fn, reads=(), writes=()):
        self._deps(eng, reads, writes)
        inst = fn(self.h[eng])
        self.cnt[eng] += 1
        inst.then_inc(self.sem[eng], 1)
        tok = ("e", eng, self.cnt[eng])
        self._mark(tok, reads, writes)
        return tok

    def next_dma_sem(self, eng):
        if eng == "pool":
            k = NDMA_HW + self.drr_sw
            self.drr_sw = (self.drr_sw + 1) % (NDMA - NDMA_HW)
        else:
            k = self.drr
            self.drr = (self.drr + 1) % NDMA_HW
        if self.dcnt[k] > 0:
            self._wait(eng, ("d", k, 16 * self.dcnt[k]))
        self.dcnt[k] += 1
        return k

    def dma(self, eng, out, in_, reads=(), writes=(), **kw):
        self._deps(eng, reads, writes, is_dma=True)
        k = self.next_dma_sem(eng)
        self.h[eng].dma_start(out=out, in_=in_, **kw).then_inc(self.dsem[k], 16)
        tok = ("d", k, 16 * self.dcnt[k])
        self._mark(tok, reads, writes)
        return tok

    def barrier(self):
        for eng in self.h:
            for other in self.h:
                if other != eng and self.cnt[other] > 0:
                    self._wait(eng, ("e", other, self.cnt[other]))
            for k in range(NDMA):
                if self.dcnt[k] > 0:
                    self._wait(eng, ("d", k, 16 * self.dcnt[k]))


class StopBuildOuter(Exception):
    pass


def build_program(debug=None):
    nc = bass.Bass("TRN2", target_bir_lowering=False)
    dt_in = {}

    def din(name, shape, dt=F32):
        dt_in[name] = nc.dram_tensor(name, list(shape), dt, kind="ExternalInput").ap()
        return dt_in[name]

    xa = din("xa", [T_ALL, D])
    posa = din("posa", [T_ALL], I32)
    vmask = din("vmask", [T_ALL])
    mem = din("mem", [256, D])
    g_mix = din("norm_mix_g", [D])
    w_in = din("w_in", [D, 1568])
    lam_re = din("ssm_lam_re", [32, 64])
    lam_im = din("ssm_lam_im", [32, 64])
    log_dt = din("ssm_log_dt", [32])
    b_re = din("ssm_b_re", [32, 64, 16])
    b_im = din("ssm_b_im", [32, 64, 16])
    c_re = din("ssm_c_re", [32, 16, 64])
    c_im = din("ssm_c_im", [32, 16, 64])
    ssm_d = din("ssm_d", [512])
    w_glu = din("ssm_w_glu", [512, 512])
    b_glu = din("ssm_b_glu", [512])
    g_q = din("mla_q_norm_g", [768])
    w_qup = din("mla_w_q_up", [768, 768])
    g_kv = din("mla_kv_norm_g", [256])
    w_kvup = din("mla_w_kv_up", [256, 1024])
    g_os = din("out_norm_ssm_g", [512])
    g_om = din("out_norm_mla_g", [512])
    w_out = din("w_out", [D, D])
    g_xa = din("norm_xattn_g", [D])
    g_mem = din("norm_mem_g", [D])
    xw_q = din("xattn_w_q", [D, D])
    xw_k = din("xattn_w_k", [D, D])
    xw_v = din("xattn_w_v", [D, D])
    xw_o = din("xattn_w_o", [D, D])
    g_moe = din("norm_moe_g", [D])
    w_group = din("moe_w_group", [D, 4])
    b_group = din("moe_b_group", [4])
    w_expert = din("moe_w_expert", [D, 32])
    b_expert = din("moe_b_expert", [32])
    wg_l = din("wg_l", [4096, 2048])
    wu_l = din("wu_l", [4096, 2048])
    wd_l = din("wd_l", [4096, 2048])
    g_fin = din("norm_final_g", [D])
    out = nc.dram_tensor("out", [T_OWN, D], F32, kind="ExternalOutput").ap()
    dbg = None
    if debug is not None:
        dbg = nc.dram_tensor("dbg", list(debug), F32, kind="ExternalOutput").ap()

    es = ExitStack()
    with es:
        try:
            S = Sync(nc, es)
            es.enter_context(nc.allow_low_precision("bf16 matmul operand staging; fp32 accumulation"))

            def sb(name, shape, dt=F32):
                t = es.enter_context(nc.sbuf_tensor(name, list(shape), dt))
                return t, Buf(name)

            PS = []
            for i in range(8):
                t = es.enter_context(nc.psum_tensor("ps%d" % i, [128, 512], F32))
                PS.append((t, Buf("ps%d" % i)))

            def mm(ps_ap, lhsT, rhs, start, stop, reads, wbuf):
                S.op("pe", lambda e: e.matmul(ps_ap, lhsT, rhs, start=start, stop=stop), reads=reads, writes=[wbuf])

            ident_f, ident_fb = sb("ident_f", [128, 128], F32)
            ident, identb = sb("ident", [128, 128], BF16)
            ones_bf, ones_bfb = sb("ones_bf", [128, 128], BF16)
            S.op("pool", lambda e: e.memset(ident_f[:], 1.0), writes=[ident_fb])
            S.op("pool", lambda e: e.affine_select(out=ident_f[:], in_=ident_f[:], pattern=[[-1, 128]],
                                                   compare_op=ALU.is_equal, fill=0.0, base=0, channel_multiplier=1),
                 reads=[ident_fb], writes=[ident_fb])
            S.op("dve", lambda e: e.tensor_copy(out=ident[:], in_=ident_f[:]), reads=[ident_fb], writes=[identb])
            S.op("pool", lambda e: e.memset(ones_bf[:], 1.0), writes=[ones_bfb])

            def bcast_load(name, src, n):
                t, b = sb(name, [128, n], F32)
                S.dma("sp", t[:], src.partition_broadcast(128), writes=[b])
                return t, b

            gmix_b, gmix_bb = bcast_load("gmix_b", g_mix, D)

            def col_load(name, src, nchunk):
                t, b = sb(name, [128, nchunk], F32)
                with nc.allow_non_contiguous_dma("small param column load"):
                    S.dma("sp", t[:], src.rearrange("(c p) -> p c", p=128), writes=[b])
                return t, b

            STACKS = []

            def newstack():
                s_ = ExitStack()
                STACKS.append(s_)
                return s_

            class StopBuild(StopBuildOuter):
                pass

            def stop_here(tag, var=None, varb=None):
                import os
                if os.environ.get("KSTOP", "") != tag:
                    return
                S.barrier()
                if var is not None and debug is not None:
                    dst_ = newstack()
                    dtmp, dtmpb = sbp(dst_, "dtmp_" + tag, [128, 4, 512], F32)
                    S.op("dve", lambda e: e.tensor_copy(out=dtmp[:], in_=var), reads=[varb], writes=[dtmpb])
                    tok = S.dma("sp", dbg.rearrange("(c p) t -> p c t", p=128), dtmp[:], reads=[dtmpb])
                    S._wait("sp", tok)
                S.barrier()
                for s_ in reversed(STACKS):
                    s_.close()
                raise StopBuild()

            def sbp(ph, name, shape, dt=F32):
                t = ph.enter_context(nc.sbuf_tensor(name, list(shape), dt))
                return t, Buf(name)

            def rsqrt_to(out_ap, outb, in_ap, inb, scale, tmp_ap, tmpb, npart=128):
                S.op("act", lambda e: e.activation(out=tmp_ap, in_=in_ap, func=AF.Sqrt, bias=eps_c[0:npart, :], scale=scale),
                     reads=[inb, eps_cb], writes=[tmpb])
                S.op("dve", lambda e: e.reciprocal(out=out_ap, in_=tmp_ap), reads=[tmpb], writes=[outb])

            eps_c, eps_cb = sb("eps_c", [128, 1], F32)
            sinb_c, sinb_cb = sb("sinb_c", [128, 2], F32)
            S.op("pool", lambda e: e.memset(sinb_c[:, 0:1], 0.0), writes=[sinb_cb])
            S.op("pool", lambda e: e.memset(sinb_c[:, 1:2], 0.25 * SIN_SCALE), reads=[sinb_cb], writes=[sinb_cb])
            S.op("pool", lambda e: e.memset(eps_c[:], EPS), writes=[eps_cb])
            MOE_TS = 256
            MOE_NT = 32 + 4096 // MOE_TS
            xscr = nc.dram_tensor("xe_scr", [MOE_NT * MOE_TS, D], BF16, kind="Internal").ap()
            ztile, ztileb = sb("ztile", [128, D], BF16)
            S.op("pool", lambda e: e.memset(ztile[:], 0.0), writes=[ztileb])
            swapm, swapmb = sb("swapm", [128, 128], BF16)
            S.op("dve", lambda e: e.tensor_copy(out=swapm[:, 0:64], in_=ident_f[:, 64:128]), reads=[ident_fb], writes=[swapmb])
            S.op("dve", lambda e: e.tensor_copy(out=swapm[:, 64:128], in_=ident_f[:, 0:64]), reads=[ident_fb], writes=[swapmb])

            stackY = newstack()
            ymT, ymTb = sbp(stackY, "ymT", [128, 4, T_OWN], BF16)
            ysT, ysTb = sbp(stackY, "ysT", [128, 4, T_OWN], BF16)
            sqn, sqnb = sbp(stackY, "sqn", [128, 6, 512], BF16)
            rtmp, rtmpb = sbp(stackY, "rtmp", [128, 512], F32)
            stackU = newstack()
            uT, uTb = sbp(stackU, "uT", [128, 4, 8, T_ALL // 8], BF16)

            pam = newstack()
            ckvT, ckvTb = sbp(pam, "ckvT", [128, 2, T_ALL], BF16)
            cqT, cqTb = sbp(pam, "cqT", [128, 6, T_OWN], BF16)
            rkvb, rkvbb = sbp(pam, "rkvb", [128, T_ALL], BF16)
            rkvt, rkvtb = sbp(pam, "rkvt", [128, 32], F32)
            rqb, rqbb = sbp(pam, "rqb", [128, T_OWN], BF16)
            cs, csb = sbp(pam, "cs", [128, T_ALL], BF16)
            kpeb = Buf("kpe")
            vmt, vmtb = sbp(pam, "vmt", [128, 32], F32)
            gkv_c, gkv_cb = sbp(pam, "gkv_c", [128, 2], F32)
            gq_c, gq_cb = sbp(pam, "gq_c", [128, 6], F32)
            with nc.allow_non_contiguous_dma("small param loads"):
                S.dma("sp", vmt[:], vmask.rearrange("(t p) -> p t", p=128), writes=[vmtb])
                S.dma("sp", gkv_c[:], g_kv.rearrange("(c p) -> p c", p=128), writes=[gkv_cb])
                S.dma("sp", gq_c[:], g_q.rearrange("(c p) -> p c", p=128), writes=[gq_cb])

            pa = newstack()
            win, winb = sbp(pa, "win", [128, 8, 1600], BF16)
            for c in range(8):
                S.dma("pool", win[:, c, 0:1568], w_in[c * 128:(c + 1) * 128, :], writes=[winb])
            S.op("dve", lambda e: e.tensor_scalar(out=win[:, :, 1568:1584], in0=win[:, :, 1552:1568], scalar1=-1.0,
                                                  scalar2=None, op0=ALU.mult), reads=[winb], writes=[winb])
            S.op("dve", lambda e: e.tensor_copy(out=win[:, :, 1584:1600], in_=win[:, :, 1536:1552]), reads=[winb], writes=[winb])

            pcs = newstack()
            ri, rib = sbp(pcs, "ri", [64, 1], I32)
            rf, rfb = sbp(pcs, "rf", [64, 4], F32)
            S.op("pool", lambda e: e.iota(ri[:], pattern=[[0, 1]], base=0, channel_multiplier=1), writes=[rib])
            S.op("dve", lambda e: e.tensor_single_scalar(out=ri[:], in_=ri[:], scalar=15, op=ALU.bitwise_and), reads=[rib], writes=[rib])
            S.op("dve", lambda e: e.tensor_copy(out=rf[:, 0:1], in_=ri[:]), reads=[rib], writes=[rfb])
            S.op("act", lambda e: e.activation(out=rf[:, 1:2], in_=rf[:, 0:1], func=AF.Exp, scale=-math.log(10000.0) / 16.0),
                 reads=[rfb], writes=[rfb])
            S.op("dve", lambda e: e.tensor_scalar(out=rf[:, 2:3], in0=rf[:, 1:2], scalar1=1.0 / TWO_PI, scalar2=None, op0=ALU.mult),
                 reads=[rfb], writes=[rfb])
            S.op("pool", lambda e: e.memset(rf[0:32, 3:4], 0.25), writes=[rfb])
            S.op("pool", lambda e: e.memset(rf[32:64, 3:4], 0.0), writes=[rfb])
            posi, posib = sbp(pcs, "posi", [64, 512], I32)
            ya, yab = sbp(pcs, "ya", [64, 512], F32)
            yb_, ybb = sbp(pcs, "yb_", [64, 512], F32)
            yc_, ycb = sbp(pcs, "yc_", [64, 512], F32)

            def range_reduce_sin(out_ap, outb, y_ap, yb, t_ap, tb, n_ap, nb, eng="dve", pre=0.0):
                if pre == 0.0:
                    S.op(eng, lambda e: e.tensor_scalar(out=t_ap, in0=y_ap, scalar1=MAGIC, scalar2=None, op0=ALU.add), reads=[yb], writes=[tb])
                else:
                    S.op(eng, lambda e: e.tensor_scalar(out=t_ap, in0=y_ap, scalar1=pre, scalar2=MAGIC, op0=ALU.add, op1=ALU.add), reads=[yb], writes=[tb])
                S.op(eng, lambda e: e.scalar_tensor_tensor(out=n_ap, in0=t_ap, scalar=-MAGIC, in1=y_ap, op0=ALU.add, op1=ALU.subtract), reads=[tb, yb], writes=[nb])
                S.op("act", lambda e: e.activation(out=out_ap, in_=n_ap, func=AF.Sin, scale=-SIN_SCALE, bias=(sinb_c[0:out_ap.shape[0], 0:1] if pre == 0.0 else sinb_c[0:out_ap.shape[0], 1:2])),
                     reads=[nb, sinb_cb], writes=[outb])

            for ch in range(8):
                S.dma("sp", posi[:], posa[ch * 512:(ch + 1) * 512].partition_broadcast(64), writes=[posib])
                S.op("dve", lambda e: e.tensor_copy(out=ya[:], in_=posi[:]), reads=[posib], writes=[yab])
                S.op("dve", lambda e: e.tensor_scalar(out=ya[:], in0=ya[:], scalar1=rf[:, 2:3], scalar2=rf[:, 3:4], op0=ALU.mult, op1=ALU.add),
                     reads=[yab, rfb], writes=[yab])
                range_reduce_sin(cs[0:64, ch * 512:(ch + 1) * 512], csb, ya[:], yab, yb_[:], ybb, yc_[:], ycb)

            S.barrier()
            pcs.close()
            xt = [sbp(pa, "xt%d" % i, [128, D], F32) for i in range(2)]
            hb = [sbp(pa, "hb%d" % i, [128, D], BF16) for i in range(2)]
            hT = [sbp(pa, "hT%d" % i, [128, 8, 512], BF16) for i in range(2)]
            sq_junk, sq_junkb = sbp(pa, "sq_junk", [128, D], BF16)
            stat = [sbp(pa, "stat%d" % i, [128, 4], F32) for i in range(4)]
            ropeA, ropeAb = sbp(pa, "ropeA", [32, 512], F32)
            ropeB, ropeBb = sbp(pa, "ropeB", [32, 512], F32)
            sst, sstb = sbp(pa, "sst", [128, 4], F32)

            def rms_rows(x_t, x_b, s_t, s_b, nfeat):
                S.op("act", lambda e: e.activation(out=sq_junk[:], in_=x_t, func=AF.Square, accum_out=s_t[:, 0:1]),
                     reads=[x_b], writes=[sq_junkb, s_b])
                S.op("act", lambda e: e.activation(out=s_t[:, 1:2], in_=s_t[:, 0:1], func=AF.Sqrt, bias=eps_c[:], scale=1.0 / nfeat),
                     reads=[s_b, eps_cb], writes=[s_b])
                S.op("dve", lambda e: e.reciprocal(out=s_t[:, 2:3], in_=s_t[:, 1:2]), reads=[s_b], writes=[s_b])

            zi = 0

            def a_stage1(tile_idx):
                x_t, x_b = xt[tile_idx % 2]
                h_t, h_b = hb[tile_idx % 2]
                s_t, s_b = stat[tile_idx % 4]
                S.dma("sp", x_t[:], xa[tile_idx * 128:(tile_idx + 1) * 128, :], writes=[x_b])
                rms_rows(x_t[:], x_b, s_t, s_b, D)
                S.op("dve", lambda e: e.scalar_tensor_tensor(out=h_t[:], in0=x_t[:], scalar=s_t[:, 2:3], in1=gmix_b[:],
                                                             op0=ALU.mult, op1=ALU.mult),
                     reads=[x_b, s_b, gmix_bb], writes=[h_b])

            def a_stage2(tile_idx, hTt, hTb, ti):
                h_t, h_b = hb[tile_idx % 2]
                pt, pb = PS[tile_idx % 2]
                ptb = pt[:].bitcast(BF16)
                for c in range(8):
                    S.op("pe", lambda e: e.transpose(ptb[:, c * 128:(c + 1) * 128], h_t[:, c * 128:(c + 1) * 128], ident[:]),
                         reads=[h_b, identb], writes=[pb])
                S.op("dve" if tile_idx % 2 else "act", (lambda e: e.tensor_copy(out=hTt[:, :, ti * 128:(ti + 1) * 128], in_=ptb.rearrange("p (c t) -> p c t", c=8))) if tile_idx % 2 else
                     (lambda e: e.copy(out=hTt[:, :, ti * 128:(ti + 1) * 128], in_=ptb.rearrange("p (c t) -> p c t", c=8))),
                     reads=[pb], writes=[hTb])

            a_stage1(0)
            for ti in range(4):
                a_stage1(ti + 1)
                a_stage2(ti, hT[0][0], hT[0][1], ti)
            for st in range(8):
                hTt, hTb = hT[st % 2]
                cols = slice(st * 512, (st + 1) * 512)
                pend_tiles = list(range(4)) if st + 1 < 8 else []

                def next_tile():
                    if pend_tiles:
                        ti = pend_tiles.pop(0)
                        tile_idx = (st + 1) * 4 + ti
                        if tile_idx + 1 < 32:
                            a_stage1(tile_idx + 1)
                        a_stage2(tile_idx, hT[(st + 1) % 2][0], hT[(st + 1) % 2][1], ti)

                def zchunk(c0, m):
                    nonlocal zi
                    pt, pb = PS[2 + (zi % 2)]
                    zi += 1
                    for k in range(8):
                        mm(pt[0:m, :], win[:, k, c0:c0 + m], hTt[:, k, :], k == 0, k == 7, [winb, hTb], pb)
                    return pt, pb

                for c in range(4):
                    pt, pb = zchunk(c * 128, 128)
                    S.op("act", lambda e: e.copy(out=uT[:, c, :, st * 64:(st + 1) * 64], in_=pt[:].rearrange("p (m j) -> p j m", j=8)), reads=[pb], writes=[uTb])
                    if c % 2 == 1:
                        next_tile()
                for c in range(2):
                    pt, pb = zchunk(1280 + c * 128, 128)
                    S.op("act", lambda e: e.activation(out=ckvT[:, c, cols], in_=pt[:], func=AF.Copy, scale=gkv_c[:, c:c + 1]),
                         reads=[pb, gkv_cb], writes=[ckvTb])
                    S.op("act", lambda e: e.activation(out=sqn[:, c, :], in_=pt[:], func=AF.Square), reads=[pb], writes=[sqnb])
                pss, pssb = PS[4]
                for c in range(2):
                    mm(pss[:], ones_bf[:], sqn[:, c, :], c == 0, c == 1, [ones_bfb, sqnb], pssb)
                rsqrt_to(rkvb[:, cols], rkvbb, pss[:], pssb, 1.0 / 256, rtmp[:], rtmpb)
                pst, pstb = PS[6]
                for ti in range(4):
                    for c in range(2):
                        mm(pst[:, ti:ti + 1], sqn[:, c, ti * 128:(ti + 1) * 128], ones_bf[:, 0:1], c == 0, c == 1, [ones_bfb, sqnb], pstb)
                rsqrt_to(rkvt[:, st * 4:(st + 1) * 4], rkvtb, pst[:, 0:4], pstb, 1.0 / 256, sst[:], sstb)
                next_tile()
                pt, pb = zchunk(1536, 64)
                S.op("dve", lambda e: e.tensor_tensor(out=ropeA[:], in0=pt[0:32, :], in1=cs[0:32, cols], op=ALU.mult), reads=[pb, csb], writes=[ropeAb])
                S.op("dve", lambda e: e.tensor_tensor(out=ropeB[:], in0=pt[32:64, :], in1=cs[32:64, cols], op=ALU.mult), reads=[pb, csb], writes=[ropeBb])
                S.op("dve", lambda e: e.tensor_tensor(out=cs[64:96, cols], in0=ropeA[:], in1=ropeB[:], op=ALU.add), reads=[ropeAb, ropeBb], writes=[kpeb])
                if st >= 4:
                    ocols = slice((st - 4) * 512, (st - 3) * 512)
                    for c in range(6):
                        pt, pb = zchunk(512 + c * 128, 128)
                        S.op("act", lambda e: e.activation(out=cqT[:, c, ocols], in_=pt[:], func=AF.Copy, scale=gq_c[:, c:c + 1]),
                             reads=[pb, gq_cb], writes=[cqTb])
                        S.op("act", lambda e: e.activation(out=sqn[:, c, :], in_=pt[:], func=AF.Square), reads=[pb], writes=[sqnb])
                    for c in range(6):
                        mm(pss[:], ones_bf[:], sqn[:, c, :], c == 0, c == 5, [ones_bfb, sqnb], pssb)
                    rsqrt_to(rqb[:, ocols], rqbb, pss[:], pssb, 1.0 / 768, rtmp[:], rtmpb)
                while pend_tiles:
                    next_tile()
            stop_here("A")
            S.barrier()
            pa.close()
            pm = newstack()
            wkv, wkvb = sbp(pm, "wkv", [128, 2, 8, 128], BF16)
            for c in range(2):
                S.dma("pool", wkv[:, c, :, :].rearrange("p h d -> p (h d)"), w_kvup[c * 128:(c + 1) * 128, :], writes=[wkvb])
            wq, wqb = sbp(pm, "wq", [128, 6, 8, 128], BF16)
            pw_ = newstack()
            wqs, wqsb = sbp(pw_, "wqs", [128, 6, 8, 96], BF16)
            for c in range(6):
                S.dma("pool", wqs[:, c, :, :].rearrange("p h d -> p (h d)"), w_qup[c * 128:(c + 1) * 128, :], writes=[wqsb])
            S.op("dve", lambda e: e.tensor_copy(out=wq[:, :, :, 0:32], in_=wqs[:, :, :, 64:96]), reads=[wqsb], writes=[wqb])
            S.op("dve", lambda e: e.tensor_scalar(out=wq[:, :, :, 32:48], in0=wqs[:, :, :, 80:96], scalar1=-1.0, scalar2=None, op0=ALU.mult),
                 reads=[wqsb], writes=[wqb])
            S.op("dve", lambda e: e.tensor_copy(out=wq[:, :, :, 48:64], in_=wqs[:, :, :, 64:80]), reads=[wqsb], writes=[wqb])
            S.op("dve", lambda e: e.tensor_copy(out=wq[:, :, :, 64:128], in_=wqs[:, :, :, 0:64]), reads=[wqsb], writes=[wqb])
            S.barrier()
            pw_.close()
            KT = [sbp(pm, "KT%d" % i, [128, T_ALL], BF16) for i in range(2)]
            VA = [sbp(pm, "VA%d" % i, [128, 32, 128], BF16) for i in range(2)]
            QT = [sbp(pm, "QT%d" % i, [128, T_OWN], BF16) for i in range(2)]
            PT = [sbp(pm, "PT%d" % i, [128, 512], BF16) for i in range(4)]
            SCB = [PS[0], PS[1], PS[7]]
            qA, qAb = sbp(pm, "qA", [32, 512], F32)
            qB, qBb = sbp(pm, "qB", [32, 512], F32)
            rl, rlb = sbp(pm, "rl", [64, 512], F32)
            S.op("dve", lambda e: e.tensor_tensor(out=cs[0:64, T_OWN:T_ALL], in0=cs[0:64, T_OWN:T_ALL], in1=rqb[0:64, :], op=ALU.mult), reads=[csb, rqbb], writes=[csb])
            csq, csqb = cs[:, T_OWN:T_ALL], csb
            for i in range(2):
                S.op("pool", lambda e: e.memset(KT[i][0][32:64, :], 0.0), writes=[KT[i][1]])
                S.op("pool", lambda e: e.memset(QT[i][0][32:64, :], 0.0), writes=[QT[i][1]])
                S.op("dve", lambda e: e.tensor_copy(out=VA[i][0][:, :, 64:128], in_=vmt[:, :].unsqueeze(2).broadcast_to([128, 32, 64])),
                     reads=[vmtb], writes=[VA[i][1]])
            zero_toks = []
            for r0 in range(0, MOE_NT * MOE_TS, 1024):
                zero_toks.append(S.dma("sp", xscr[r0:r0 + 1024, :].rearrange("(s p) d -> p s d", p=128),
                                       ztile[:, :].unsqueeze(1).broadcast_to([128, 8, D]), reads=[ztileb]))
            wbf = [nc.dram_tensor("wbf%d" % i, [4096, 2048], BF16, kind="Internal").ap() for i in range(3)]
            conv_toks = []
            for r0 in range(0, 4096, 512):
                for i, src in enumerate((wg_l, wu_l, wd_l)):
                    conv_toks.append(S.dma("pool", wbf[i][r0:r0 + 512, :], src[r0:r0 + 512, :]))
            SCALE = 96.0 ** -0.5
            pti = 0
            psi = 0
            def build_K(h):
                KTt, KTb = KT[h % 2]
                VAt, VAb = VA[h % 2]
                QTt, QTb = QT[h % 2]
                S.op("act", lambda e: e.copy(out=KTt[0:32, :], in_=cs[64:96, :]), reads=[kpeb], writes=[KTb])
                for st in range(8):
                    cols = slice(st * 512, (st + 1) * 512)
                    pk, pkb = PS[4] if st % 2 == 0 else PS[6]
                    for c in range(2):
                        mm(pk[:, :], wkv[:, c, h, :], ckvT[:, c, cols], c == 0, c == 1, [wkvb, ckvTb], pkb)
                    S.op("dve", lambda e: e.tensor_tensor(out=KTt[64:128, cols], in0=pk[0:64, :], in1=rkvb[0:64, cols], op=ALU.mult),
                         reads=[pkb, rkvbb], writes=[KTb])

            def build_V(h):
                KTt, KTb = KT[h % 2]
                VAt, VAb = VA[h % 2]
                QTt, QTb = QT[h % 2]
                for kg in range(4):
                    pv, pvb = PS[5]
                    for j in range(8):
                        kt = kg * 8 + j
                        for c in range(2):
                            mm(pv[:, j * 64:(j + 1) * 64], ckvT[:, c, kt * 128:(kt + 1) * 128], wkv[:, c, h, 64:128], c == 0, c == 1,
                               [wkvb, ckvTb], pvb)
                    S.op("dve", lambda e: e.tensor_tensor(out=VAt[:, kg * 8:(kg + 1) * 8, 0:64],
                                                          in0=pv[:].rearrange("p (j d) -> p j d", j=8),
                                                          in1=rkvt[:, kg * 8:(kg + 1) * 8].unsqueeze(2).broadcast_to([128, 8, 64]), op=ALU.mult),
                         reads=[pvb, rkvtb], writes=[VAb])

            def build_Q(h):
                KTt, KTb = KT[h % 2]
                VAt, VAb = VA[h % 2]
                QTt, QTb = QT[h % 2]
                for st in range(4):
                    cols = slice(st * 512, (st + 1) * 512)
                    pq, pqb = PS[6] if st % 2 == 0 else PS[4]
                    for c in range(6):
                        mm(pq[:], wq[:, c, h, :], cqT[:, c, cols], c == 0, c == 5, [wqb, cqTb], pqb)
                    S.op("dve", lambda e: e.tensor_tensor(out=QTt[64:128, cols], in0=pq[64:128, :], in1=rqb[64:128, cols], op=ALU.mult),
                         reads=[pqb, rqbb], writes=[QTb])
                    S.op("dve", lambda e: e.tensor_tensor(out=qA[:], in0=pq[0:32, :], in1=csq[0:32, cols], op=ALU.mult), reads=[pqb, csqb], writes=[qAb])
                    S.op("dve", lambda e: e.tensor_tensor(out=qB[:], in0=pq[32:64, :], in1=csq[32:64, cols], op=ALU.mult), reads=[pqb, csqb], writes=[qBb])
                    S.op("dve", lambda e: e.tensor_tensor(out=QTt[0:32, cols], in0=qA[:], in1=qB[:], op=ALU.add), reads=[qAb, qBb], writes=[QTb])


            def build_head(h):
                build_K(h)
                build_V(h)
                build_Q(h)

            def attn_head(h, st):
                nonlocal psi, pti
                KTt, KTb = KT[h % 2]
                VAt, VAb = VA[h % 2]
                QTt, QTb = QT[h % 2]
                po, pob = PS[2 + ((h * 4 + st) % 2)]
                nkb = 16 + 4 * st + 4
                pend = []
                for kb in range(nkb):
                    j = kb - (16 + 4 * st)
                    c0 = 128 * j if j > 0 else 0
                    pss_, pssb_ = SCB[psi % 3]
                    psi += 1
                    P_t, P_b = PT[pti % 4]
                    pti += 1
                    mm(pss_[:, c0:512], KTt[:, kb * 128:(kb + 1) * 128], QTt[:, st * 512 + c0:(st + 1) * 512], True, True, [KTb, QTb], pssb_)
                    S.op("act", lambda e: e.activation(out=P_t[:, c0:512], in_=pss_[:, c0:512], func=AF.Exp, scale=SCALE), reads=[pssb_], writes=[P_b])
                    if j >= 0:
                        S.op("dve", lambda e: e.memset(P_t[64:128, c0:c0 + 64], 0.0), writes=[P_b])
                    pend.append((kb, c0, P_t, P_b))
                    if len(pend) > 2:
                        pkb, pc0, pP_t, pP_b = pend.pop(0)
                        mm(po[:, pc0:512], VAt[:, pkb, :], pP_t[:, pc0:512], pkb == 0, False, [VAb, pP_b], pob)
                while pend:
                    pkb, pc0, pP_t, pP_b = pend.pop(0)
                    mm(po[:, pc0:512], VAt[:, pkb, :], pP_t[:, pc0:512], pkb == 0, len(pend) == 0, [VAb, pP_b], pob)
                S.op("dve", lambda e: e.reciprocal(out=rl[:], in_=po[64:128, :]), reads=[pob], writes=[rlb])
                r0 = (h % 2) * 64
                S.op("dve", lambda e: e.tensor_tensor(out=ymT[r0:r0 + 64, h // 2, st * 512:(st + 1) * 512], in0=po[0:64, :], in1=rl[:], op=ALU.mult),
                     reads=[pob, rlb], writes=[ymTb])

            build_head(0)
            for h in range(8):
                for st in range(4):
                    attn_head(h, st)
                    if st == 1 and h + 1 < 8:
                        build_head(h + 1)
            gom_c, gom_cb = sbp(pm, "gom_c", [128, 4], F32)
            with nc.allow_non_contiguous_dma("small param loads"):
                S.dma("sp", gom_c[:], g_om.rearrange("(c p) -> p c", p=128), writes=[gom_cb])

            def branch_norm(yT, yTb, gc, gcb):
                for st in range(4):
                    cols = slice(st * 512, (st + 1) * 512)
                    S.op("act", lambda e: e.activation(out=sqn[:, 0:4, :], in_=yT[:, :, cols], func=AF.Square), reads=[yTb], writes=[sqnb])
                    pn, pnb = PS[7]
                    for c in range(4):
                        mm(pn[:], ones_bf[:], sqn[:, c, :], c == 0, c == 3, [ones_bfb, sqnb], pnb)
                    rsqrt_to(rtmp[:], rtmpb, pn[:], pnb, 1.0 / 512, rtmp[:], rtmpb)
                    for c in range(4):
                        S.op("dve", lambda e: e.scalar_tensor_tensor(out=yT[:, c, cols], in0=yT[:, c, cols], scalar=gc[:, c:c + 1], in1=rtmp[:],
                                                                     op0=ALU.mult, op1=ALU.mult), reads=[yTb, gcb, rtmpb], writes=[yTb])

            branch_norm(ymT, ymTb, gom_c, gom_cb)
            stop_here("M")
            S.barrier()
            pm.close()
            pam.close()
            ps_ = newstack()
            WS, WSb = sbp(ps_, "WS", [128, 32, 128], BF16)
            WSs, WSsb = sbp(ps_, "WSs", [128, 32, 128], BF16)
            WY, WYb = sbp(ps_, "WY", [128, 32, 128], BF16)
            WT, WTb = sbp(ps_, "WT", [128, 32, 128], BF16)
            Est, Estb = sbp(ps_, "Est", [128, 8, 8, 128], BF16)
            th8, th8b = sbp(ps_, "th8", [128, 32], F32)
            r8, r8b = sbp(ps_, "r8", [128, 32], F32)
            S.op("pool", lambda e: e.memset(Est[:], 1.0), writes=[Estb])
            S.op("pool", lambda e: e.affine_select(out=Est[:], in_=Est[:], pattern=[[-16, 8], [0, 8], [0, 8], [-1, 16]], compare_op=ALU.is_equal,
                                                   fill=0.0, base=0, channel_multiplier=1), reads=[Estb], writes=[Estb])
            S.op("pool", lambda e: e.affine_select(out=Est[:], in_=Est[:], pattern=[[0, 8], [1, 8], [-1, 8], [0, 16]], compare_op=ALU.is_equal,
                                                   fill=0.0, base=0, channel_multiplier=0), reads=[Estb], writes=[Estb])
            stop_here("S0")
            pp = newstack()
            lre, lreb = sbp(pp, "lre", [128, 32], F32)
            lim, limb = sbp(pp, "lim", [128, 32], F32)
            dtt, dttb = sbp(pp, "dtt", [128, 32], F32)
            Bre, Breb = sbp(pp, "Bre", [128, 32, 16], F32)
            Bim, Bimb = sbp(pp, "Bim", [128, 32, 16], F32)
            Cre, Creb = sbp(pp, "Cre", [128, 32, 16], F32)
            Cim, Cimb = sbp(pp, "Cim", [128, 32, 16], F32)
            cld, cldb = sbp(pp, "cld", [128, 4, 64], F32)
            dcol, dcolb = sbp(pp, "dcol", [128, 32], F32)
            with nc.allow_non_contiguous_dma("small ssm param loads"):
                for hf in range(2):
                    S.dma("sp", lre[hf * 64:(hf + 1) * 64, :], lam_re.rearrange("g p -> p g"), writes=[lreb])
                    S.dma("sp", lim[hf * 64:(hf + 1) * 64, :], lam_im.rearrange("g p -> p g"), writes=[limb])
                    S.dma("sp", Bre[hf * 64:(hf + 1) * 64, :, :], b_re.rearrange("g p h -> p g h"), writes=[Breb])
                    S.dma("sp", Bim[hf * 64:(hf + 1) * 64, :, :], b_im.rearrange("g p h -> p g h"), writes=[Bimb])
                for j in range(8):
                    S.dma("sp", dcol[j * 16:(j + 1) * 16, :], ssm_d.rearrange("(g h) -> h g", h=16), writes=[dcolb])
            S.dma("sp", dtt[:], log_dt.partition_broadcast(128), writes=[dttb])
            for (src, dst, dstb) in ((c_re, Cre, Creb), (c_im, Cim, Cimb)):
                S.dma("sp", cld[:], src.rearrange("g h p -> (g h) p").rearrange("(t q) p -> q t p", q=128), writes=[cldb])
                pc, pcb = PS[0]
                for t in range(4):
                    S.op("pe", lambda e: e.transpose(pc[0:64, t * 128:(t + 1) * 128], cld[:, t, :], ident_f[:]), reads=[cldb, ident_fb], writes=[pcb])
                S.op("dve", lambda e: e.tensor_copy(out=dst[0:64, :, :].rearrange("p g h -> p (g h)"), in_=pc[0:64, :]), reads=[pcb], writes=[dstb])
                S.op("dve", lambda e: e.tensor_copy(out=dst[64:128, :, :].rearrange("p g h -> p (g h)"), in_=pc[0:64, :]), reads=[pcb], writes=[dstb])
            S.op("act", lambda e: e.activation(out=dtt[:], in_=dtt[:], func=AF.Exp), reads=[dttb], writes=[dttb])
            S.op("dve", lambda e: e.tensor_scalar(out=lre[:], in0=lre[:], scalar1=-1e-4, scalar2=None, op0=ALU.min), reads=[lreb], writes=[lreb])
            stop_here("S1")
            x1, x1b = sbp(pp, "x1", [128, 32], F32)
            a1, a1b = sbp(pp, "a1", [128, 32], F32)
            S.op("dve", lambda e: e.tensor_tensor(out=x1[:], in0=lre[:], in1=dtt[:], op=ALU.mult), reads=[lreb, dttb], writes=[x1b])
            S.op("dve", lambda e: e.tensor_tensor(out=a1[:], in0=lim[:], in1=dtt[:], op=ALU.mult), reads=[limb, dttb], writes=[a1b])
            S.op("dve", lambda e: e.tensor_scalar(out=a1[:], in0=a1[:], scalar1=1.0 / TWO_PI, scalar2=None, op0=ALU.mult), reads=[a1b], writes=[a1b])
            kvi, kvib = sbp(pp, "kvi", [128, 16], I32)
            kv, kvb_ = sbp(pp, "kv", [128, 16], F32)
            S.op("pool", lambda e: e.iota(kvi[:, 0:8], pattern=[[1, 8]], base=1, channel_multiplier=0), writes=[kvib])
            S.op("pool", lambda e: e.iota(kvi[:, 8:16], pattern=[[-1, 8]], base=-1, channel_multiplier=0), reads=[kvib], writes=[kvib])
            S.op("dve", lambda e: e.tensor_copy(out=kv[:], in_=kvi[:]), reads=[kvib], writes=[kvb_])
            KX, KXb = sbp(pp, "KX", [128, 16, 32], F32)
            MAG, MAGb = sbp(pp, "MAGt", [128, 16, 32], F32)
            YY, YYb = sbp(pp, "YY", [128, 16, 32], F32)
            Y2, Y2b = sbp(pp, "Y2", [128, 16, 32], F32)
            T1, T1b = sbp(pp, "T1", [128, 16, 32], F32)
            T2, T2b = sbp(pp, "T2", [128, 16, 32], F32)
            PWr, PWrb = sbp(pp, "PWr", [128, 16, 32], F32)
            PWi, PWib = sbp(pp, "PWi", [128, 16, 32], F32)
            kvB = kv[:, :].unsqueeze(2).broadcast_to([128, 16, 32])
            S.op("dve", lambda e: e.tensor_tensor(out=KX[:], in0=kvB, in1=x1[:, :].unsqueeze(1).broadcast_to([128, 16, 32]), op=ALU.mult),
                 reads=[kvb_, x1b], writes=[KXb])
            S.op("act", lambda e: e.activation(out=MAG[:], in_=KX[:], func=AF.Exp), reads=[KXb], writes=[MAGb])
            S.op("dve", lambda e: e.tensor_tensor(out=YY[:], in0=kvB, in1=a1[:, :].unsqueeze(1).broadcast_to([128, 16, 32]), op=ALU.mult),
                 reads=[kvb_, a1b], writes=[YYb])
            fl = lambda t: t[:, :, :].rearrange("p a b -> p (a b)")
            range_reduce_sin(fl(PWi), PWib, fl(YY), YYb, fl(T1), T1b, fl(T2), T2b)
            range_reduce_sin(fl(PWr), PWrb, fl(YY), YYb, fl(T1), T1b, fl(T2), T2b, pre=0.25)
            S.op("dve", lambda e: e.tensor_tensor(out=PWr[:], in0=PWr[:], in1=MAG[:], op=ALU.mult), reads=[PWrb, MAGb], writes=[PWrb])
            S.op("dve", lambda e: e.tensor_tensor(out=PWi[:], in0=PWi[:], in1=MAG[:], op=ALU.mult), reads=[PWib, MAGb], writes=[PWib])
            S.op("dve", lambda e: e.tensor_copy(out=r8[:], in_=MAG[:, 7, :]), reads=[MAGb], writes=[r8b])
            S.op("dve", lambda e: e.tensor_scalar(out=th8[0:64, :], in0=a1[0:64, :], scalar1=8.0, scalar2=None, op0=ALU.mult), reads=[a1b], writes=[th8b])
            S.op("dve", lambda e: e.tensor_scalar(out=th8[64:128, :], in0=a1[64:128, :], scalar1=-8.0, scalar2=None, op0=ALU.mult), reads=[a1b], writes=[th8b])
            stop_here("S2")
            sc = [sbp(pp, "sc%d" % i, [128, 32], F32) for i in range(6)]

            def tt(o, a, b, op, eng="dve"):
                S.op(eng, lambda e: e.tensor_tensor(out=o[0], in0=a[0], in1=b[0], op=op), reads=[a[1], b[1]], writes=[o[1]])

            def V(t, b, ap=None):
                return (t[:] if ap is None else ap, b)

            are = (PWr[:, 0, :], PWrb)
            aim = (PWi[:, 0, :], PWib)
            nr_, den, rden, cre, cim, tmpc = [(sc[i][0][:], sc[i][1]) for i in range(6)]
            S.op("dve", lambda e: e.tensor_scalar(out=nr_[0], in0=are[0], scalar1=-1.0, scalar2=None, op0=ALU.add), reads=[PWrb], writes=[nr_[1]])
            tt(den, (lre[:], lreb), (lre[:], lreb), ALU.mult)
            tt(tmpc, (lim[:], limb), (lim[:], limb), ALU.mult)
            tt(den, den, tmpc, ALU.add)
            S.op("dve", lambda e: e.reciprocal(out=rden[0], in_=den[0]), reads=[den[1]], writes=[rden[1]])
            tt(cre, nr_, (lre[:], lreb), ALU.mult)
            tt(tmpc, aim, (lim[:], limb), ALU.mult)
            tt(cre, cre, tmpc, ALU.add)
            tt(cre, cre, rden, ALU.mult)
            tt(cim, aim, (lre[:], lreb), ALU.mult)
            tt(tmpc, nr_, (lim[:], limb), ALU.mult)
            tt(cim, cim, tmpc, ALU.subtract)
            tt(cim, cim, rden, ALU.mult)
            BBr, BBrb = sbp(pp, "BBr", [128, 32, 16], F32)
            BBi, BBib = sbp(pp, "BBi", [128, 32, 16], F32)
            tb1, tb1b = sbp(pp, "tb1", [128, 32, 16], F32)
            creB = (cre[0].unsqueeze(2).broadcast_to([128, 32, 16]), cre[1])
            cimB = (cim[0].unsqueeze(2).broadcast_to([128, 32, 16]), cim[1])
            tt((BBr[:], BBrb), (Bre[:], Breb), creB, ALU.mult)
            tt((tb1[:], tb1b), (Bim[:], Bimb), cimB, ALU.mult)
            tt((BBr[:], BBrb), (BBr[:], BBrb), (tb1[:], tb1b), ALU.subtract)
            tt((BBi[:], BBib), (Bre[:], Breb), cimB, ALU.mult)
            tt((tb1[:], tb1b), (Bim[:], Bimb), creB, ALU.mult)
            tt((BBi[:], BBib), (BBi[:], BBib), (tb1[:], tb1b), ALU.add)
            stop_here("S3")
            NGH = 8
            big = [sbp(pp, "big%d" % i, [128, NGH, 8, 16], F32) for i in range(8)]
            G1, G2, G3, G4, G5, G6, G7, G8 = [(b[0][:], b[1]) for b in big]
            L2b, L2bb = sbp(pp, "L2b", [128, NGH, 128], BF16)
            Vb, Vbb = sbp(pp, "Vb", [128, NGH, 128], BF16)
            Vs, Vsb = sbp(pp, "Vs", [128, NGH, 128], BF16)
            wtf, wtfb = sbp(pp, "wtf", [128, 128], F32)

            def cmul(outr, outi, ar, ai, br, bi, t1, t2):
                t3_, t4_ = G7, G8
                tt(t1, ar, br, ALU.mult)
                tt(t2, ai, bi, ALU.mult)
                tt(outr, t1, t2, ALU.subtract)
                tt(t3_, ar, bi, ALU.mult, eng="pool")
                tt(t4_, ai, br, ALU.mult, eng="pool")
                tt(outi, t3_, t4_, ALU.add, eng="pool")

            def flat(ap):
                return ap.rearrange("p g j h -> p g (j h)")

            for gh in range(32 // NGH):
                g0 = gh * NGH
                gs = slice(g0, g0 + NGH)

                def pw(t, tb, lo):
                    return (t[:, lo:lo + 8, gs].rearrange("p k g -> p g k").unsqueeze(3).broadcast_to([128, NGH, 8, 16]), tb)

                def bj(t, tb):
                    return (t[:, gs, :].unsqueeze(2).broadcast_to([128, NGH, 8, 16]), tb)

                cmul(G1, G2, bj(Cre, Creb), bj(Cim, Cimb), pw(PWr, PWrb, 0), pw(PWi, PWib, 0), G3, G4)
                S.op("act", lambda e: e.copy(out=WY[0:64, gs, :], in_=flat(G1[0])[0:64]), reads=[G1[1]], writes=[WYb])
                S.op("act", lambda e: e.mul(out=WY[64:128, gs, :], in_=flat(G2[0])[64:128], mul=-1.0), reads=[G2[1]], writes=[WYb])
                cmul(G1, G2, pw(PWr, PWrb, 8), pw(PWi, PWib, 8), bj(BBr, BBrb), bj(BBi, BBib), G3, G4)
                S.op("act", lambda e: e.copy(out=L2b[0:64, :, :], in_=flat(G1[0])[0:64]), reads=[G1[1]], writes=[L2bb])
                S.op("act", lambda e: e.copy(out=L2b[64:128, :, :], in_=flat(G2[0])[64:128]), reads=[G2[1]], writes=[L2bb])
                a8r = (PWr[:, 7, gs].unsqueeze(2).unsqueeze(3).broadcast_to([128, NGH, 8, 16]), PWrb)
                a8i = (PWi[:, 7, gs].unsqueeze(2).unsqueeze(3).broadcast_to([128, NGH, 8, 16]), PWib)
                cmul(G5, G6, a8r, a8i, G1, G2, G3, G4)
                S.op("act", lambda e: e.copy(out=Vb[0:64, :, :], in_=flat(G5[0])[0:64]), reads=[G5[1]], writes=[Vbb])
                S.op("act", lambda e: e.copy(out=Vb[64:128, :, :], in_=flat(G6[0])[64:128]), reads=[G6[1]], writes=[Vbb])
                S.op("act", lambda e: e.copy(out=Vs[0:64, :, :], in_=flat(G6[0])[0:64]), reads=[G6[1]], writes=[Vsb])
                S.op("act", lambda e: e.copy(out=Vs[64:128, :, :], in_=flat(G5[0])[64:128]), reads=[G5[1]], writes=[Vsb])
                for gg in range(NGH):
                    g = g0 + gg
                    pa_, pab_ = PS[g % 2]
                    pab16 = pa_[:].bitcast(BF16)
                    S.op("pe", lambda e: e.transpose(pab16[:, 0:128], Vb[:, gg, :], ident[:]), reads=[Vbb, identb], writes=[pab_])
                    S.op("pe", lambda e: e.transpose(pab16[:, 128:256], Vs[:, gg, :], ident[:]), reads=[Vsb, identb], writes=[pab_])
                    S.op("act", lambda e: e.copy(out=WS[:, g, :], in_=pab16[:, 0:128]), reads=[pab_], writes=[WSb])
                    S.op("act", lambda e: e.copy(out=WSs[:, g, :], in_=pab16[:, 128:256]), reads=[pab_], writes=[WSsb])
                    pw_t, pwb_ = PS[2 + (g % 2)]
                    mm(pw_t[:, 0:128], L2b[:, gg, :], WY[:, g, :], True, True, [L2bb, WYb], pwb_)
                    S.op("dve", lambda e: e.scalar_tensor_tensor(out=wtf[:], in0=ident_f[:], scalar=dcol[:, g:g + 1], in1=pw_t[:, 0:128],
                                                                 op0=ALU.mult, op1=ALU.add), reads=[ident_fb, dcolb, pwb_], writes=[wtfb])
                    S.op("pool", lambda e: e.affine_select(out=WT[:, g, :].rearrange("p (j h) -> p j h", j=8), in_=wtf[:].rearrange("p (j h) -> p j h", j=8),
                                                           pattern=[[16, 8], [0, 16]], compare_op=ALU.is_ge, fill=0.0, base=15, channel_multiplier=-1),
                         reads=[wtfb], writes=[WTb])
            stop_here("S4")
            S.barrier()
            pp.close()
            Eun, Eunb = sbp(ps_, "Eun", [128, 8, 8, 128], BF16)
            S.op("pool", lambda e: e.memset(Eun[:], 1.0), writes=[Eunb])
            S.op("pool", lambda e: e.affine_select(out=Eun[:], in_=Eun[:], pattern=[[0, 8], [-16, 8], [0, 8], [-1, 16]], compare_op=ALU.is_equal,
                                                   fill=0.0, base=0, channel_multiplier=1), reads=[Eunb], writes=[Eunb])
            S.op("pool", lambda e: e.affine_select(out=Eun[:], in_=Eun[:], pattern=[[1, 8], [0, 8], [-1, 8], [0, 16]], compare_op=ALU.is_equal,
                                                   fill=0.0, base=0, channel_multiplier=0), reads=[Eunb], writes=[Eunb])
            pl = newstack()
            iot, iotb = sbp(pl, "iot", [128, 512], F32)
            TcB = [sbp(pl, "Tc%d" % i, [128, 2, 512], F32) for i in range(2)]
            TsB = [sbp(pl, "Ts%d" % i, [128, 2, 512], F32) for i in range(2)]
            Yt, Ytb = sbp(pl, "Yt", [128, 2, 512], F32)
            Yu, Yub = sbp(pl, "Yu", [128, 2, 512], F32)
            Yv, Yvb = sbp(pl, "Yv", [128, 2, 512], F32)
            Yw, Ywb = sbp(pl, "Yw", [128, 2, 512], F32)
            ioti = Yw[:, 0, :].bitcast(I32)
            S.op("pool", lambda e: e.iota(ioti, pattern=[[1, 512]], base=1, channel_multiplier=0), writes=[Ywb])
            S.op("dve", lambda e: e.tensor_copy(out=iot[:], in_=ioti), reads=[Ywb], writes=[iotb])
            Ust = [sbp(pl, "Ust%d" % i, [128, 512], BF16) for i in range(3)]
            sA = [sbp(pl, "sA%d" % i, [128, 512], F32) for i in range(2)]
            sB = [sbp(pl, "sB%d" % i, [128, 512], F32) for i in range(2)]
            St_ = [sbp(pl, "St%d" % i, [128, 512], F32) for i in range(2)]
            xs = [sbp(pl, "xs%d" % i, [128, 512], F32) for i in range(2)]
            xsb16 = [sbp(pl, "xsb%d" % i, [128, 256], BF16) for i in range(2)]
            t3 = [sbp(pl, "t3%d" % i, [128, 256], F32) for i in range(2)]
            t4 = [sbp(pl, "t4%d" % i, [128, 256], F32) for i in range(2)]
            Xb = [sbp(pl, "Xb%d" % i, [128, 256], BF16) for i in range(2)]
            Yst, Ystb = sbp(pl, "Yst", [128, 8, 256], BF16)
            ypre, ypreb = ysT, ysTb
            f4 = lambda t: t[:, :, :].rearrange("p a b -> p (a b)")
            OWN = slice(255, 511)
            def stage0(g):
                c, gl = g // 8, g % 8
                U_t, U_b = Ust[g % 3]
                pu, pub = PS[g % 2]
                uview = uT[:, c, :, :]
                for j in range(8):
                    mm(pu[:], Est[:, gl, j, :], uview[:, j, :], j == 0, j == 7, [Estb, uTb], pub)
                S.op("act", lambda e: e.copy(out=U_t[:], in_=pu[:]), reads=[pub], writes=[U_b])

            def stage1a(g):
                U_t, U_b = Ust[g % 3]
                p1, p1b = PS[2 + (g % 2)]
                p2, p2b = PS[4 + (g % 2)]
                mm(p1[:], WS[:, g, :], U_t[:], True, True, [WSb, U_b], p1b)
                mm(p2[:], WSs[:, g, :], U_t[:], True, True, [WSsb, U_b], p2b)

            def stage1b(g):
                gi = g % 2
                Tc, Tcb = TcB[(g // 2) % 2]
                Ts, Tsb = TsB[(g // 2) % 2]
                if gi == 0:
                    S.op("dve", lambda e: e.tensor_tensor(out=Yt[:], in0=iot[:, :].unsqueeze(1).broadcast_to([128, 2, 512]),
                                                          in1=th8[:, g:g + 2].unsqueeze(2).broadcast_to([128, 2, 512]), op=ALU.mult),
                         reads=[iotb, th8b], writes=[Ytb])
                    range_reduce_sin(f4(Ts), Tsb, f4(Yt), Ytb, f4(Yv), Yvb, f4(Yw), Ywb)
                    range_reduce_sin(f4(Tc), Tcb, f4(Yt), Ytb, f4(Yu), Yub, f4(Yv), Yvb, pre=0.25)
                p1, p1b = PS[2 + (g % 2)]
                p2, p2b = PS[4 + (g % 2)]
                a_t, a_b = sA[g % 2]
                b_t, b_b = sB[g % 2]
                s_t, s_b = St_[g % 2]
                x_t, x_b = xs[g % 2]
                S.op("dve", lambda e: e.tensor_tensor(out=a_t[:], in0=p1[:], in1=Tc[:, gi, :], op=ALU.mult), reads=[p1b, Tcb], writes=[a_b])
                S.op("dve", lambda e: e.tensor_tensor(out=b_t[:], in0=p2[:], in1=Ts[:, gi, :], op=ALU.mult), reads=[p2b, Tsb], writes=[b_b])
                S.op("dve", lambda e: e.tensor_tensor(out=s_t[:], in0=a_t[:], in1=b_t[:], op=ALU.add), reads=[a_b, b_b], writes=[s_b])
                S.op("dve", lambda e: e.tensor_tensor_scan(out=x_t[:], data0=r8[:, g:g + 1].broadcast_to([128, 512]), data1=s_t[:], initial=0.0,
                                                           op0=ALU.mult, op1=ALU.add), reads=[r8b, s_b], writes=[x_b])
                xb_t, xb_b = xsb16[g % 2]
                S.op("act", lambda e: e.copy(out=xb_t[:], in_=x_t[:, OWN]), reads=[x_b], writes=[xb_b])

            def stage2(g):
                c, gl = g // 8, g % 8
                gi = g % 2
                Tc, Tcb = TcB[(g // 2) % 2]
                Ts, Tsb = TsB[(g // 2) % 2]
                U_t, U_b = Ust[g % 3]
                x_t, x_b = xs[g % 2]
                xb_t, xb_b = xsb16[g % 2]
                p1, p1b = PS[2 + (g % 2)]
                p3, p3b = PS[6 + (g % 2)]
                mm(p3[:, 0:256], swapm[:], xb_t[:], True, True, [swapmb, xb_b], p3b)
                t3t, t3b = t3[g % 2]
                t4t, t4b = t4[g % 2]
                X_t, X_b = Xb[g % 2]
                S.op("dve", lambda e: e.tensor_tensor(out=t3t[:], in0=x_t[:, OWN], in1=Tc[:, gi, OWN], op=ALU.mult), reads=[x_b, Tcb], writes=[t3b])
                S.op("dve", lambda e: e.tensor_tensor(out=t4t[:], in0=p3[:, 0:256], in1=Ts[:, gi, OWN], op=ALU.mult), reads=[p3b, Tsb], writes=[t4b])
                S.op("dve", lambda e: e.tensor_tensor(out=X_t[:], in0=t3t[:], in1=t4t[:], op=ALU.subtract), reads=[t3b, t4b], writes=[X_b])
                mm(p1[:, 0:256], WY[:, g, :], X_t[:], True, False, [WYb, X_b], p1b)
                mm(p1[:, 0:256], WT[:, g, :], U_t[:, 256:512], False, True, [WTb, U_b], p1b)
                S.op("act", lambda e: e.copy(out=Yst[:, gl, :], in_=p1[:, 0:256]), reads=[p1b], writes=[Ystb])
                if gl == 7:
                    yview = ypre[:, c, :].rearrange("p (m j) -> p j m", j=8)
                    for j in range(8):
                        pj, pjb = PS[j % 2]
                        for g2 in range(8):
                            mm(pj[:, 0:256], Eun[:, g2, j, :], Yst[:, g2, :], g2 == 0, g2 == 7, [Eunb, Ystb], pjb)
                        S.op("act", lambda e: e.copy(out=yview[:, j, :], in_=pj[:, 0:256]), reads=[pjb], writes=[ypreb])

            stage0(0)
            stage0(1)
            stage1a(0)
            stage1b(0)
            for g in range(32):
                if g + 1 < 32:
                    stage1a(g + 1)
                if g + 2 < 32:
                    stage0(g + 2)
                if g + 1 < 32:
                    stage1b(g + 1)
                stage2(g)
            stop_here("S5", ysT[:, :, 0:512], ysTb)
            S.barrier()
            pl.close()
            pl = newstack()
            wglu, wglub = sbp(pl, "wglu", [128, 4, 512], BF16)
            for c in range(4):
                S.dma("pool", wglu[:, c, :], w_glu[c * 128:(c + 1) * 128, :], writes=[wglub])
            bglu_c, bglu_cb = sbp(pl, "bglu_c", [128, 4], F32)
            gos_c, gos_cb = sbp(pl, "gos_c", [128, 4], F32)
            with nc.allow_non_contiguous_dma("small param loads"):
                S.dma("sp", bglu_c[:], b_glu.rearrange("(c p) -> p c", p=128), writes=[bglu_cb])
                S.dma("sp", gos_c[:], g_os.rearrange("(c p) -> p c", p=128), writes=[gos_cb])
            g1, g1b = sbp(pl, "g1", [128, 4, 512], F32)
            g2_, g2b = sbp(pl, "g2", [128, 4, 512], F32)
            y1, y1b = sbp(pl, "y1", [128, 4, 512], BF16)
            sg, sgb = sbp(pl, "sg", [128, 512], F32)
            for st in range(4):
                cols = slice(st * 512, (st + 1) * 512)
                yv = ypre[:, :, cols]
                S.op("dve", lambda e: e.tensor_tensor(out=g1[:], in0=yv, in1=yv, op=ALU.mult), reads=[ypreb], writes=[g1b])
                S.op("dve", lambda e: e.tensor_scalar(out=g1[:], in0=g1[:], scalar1=0.044715, scalar2=1.0, op0=ALU.mult, op1=ALU.add), reads=[g1b], writes=[g1b])
                S.op("dve", lambda e: e.tensor_tensor(out=g2_[:], in0=g1[:], in1=yv, op=ALU.mult), reads=[g1b, ypreb], writes=[g2b])
                S.op("act", lambda e: e.activation(out=g1[:], in_=g2_[:], func=AF.Sigmoid, scale=1.5957691216057308), reads=[g2b], writes=[g1b])
                S.op("dve", lambda e: e.tensor_tensor(out=y1[:], in0=g1[:], in1=yv, op=ALU.mult), reads=[g1b, ypreb], writes=[y1b])
                for c2 in range(4):
                    pg, pgb = PS[c2 % 2]
                    for c in range(4):
                        mm(pg[:], wglu[:, c, c2 * 128:(c2 + 1) * 128], y1[:, c, :], c == 0, c == 3, [wglub, y1b], pgb)
                    S.op("act", lambda e: e.activation(out=sg[:], in_=pg[:], func=AF.Sigmoid, bias=bglu_c[:, c2:c2 + 1]), reads=[pgb, bglu_cb], writes=[sgb])
                    S.op("dve", lambda e: e.tensor_tensor(out=ysT[:, c2, cols], in0=y1[:, c2, :], in1=sg[:], op=ALU.mult), reads=[y1b, sgb], writes=[ysTb])
            stop_here("S6", ysT[:, :, 0:512], ysTb)
            branch_norm(ysT, ysTb, gos_c, gos_cb)
            stop_here("S7", ysT[:, :, 0:512], ysTb)
            S.barrier()
            pl.close()
            ps_.close()
            stackU.close()
            rest = newstack()
            xres, xresb = sbp(rest, "xres", [128, 16, D], F32)
            po_ = newstack()
            wout, woutb = sbp(po_, "wout", [128, 8, D], BF16)
            for c in range(8):
                S.dma("pool", wout[:, c, :], w_out[c * 128:(c + 1) * 128, :], writes=[woutb])
            xrb = [Buf("xres%d" % i) for i in range(16)]
            for i in range(16):
                S.dma("sp", xres[:, i, :], xa[T_OWN + i * 128:T_OWN + (i + 1) * 128, :], writes=[xrb[i]])
                for hf in range(2):
                    pt, pb = PS[(2 * i + hf) % 4]
                    for c in range(8):
                        src, srcb = (ysT, ysTb) if c < 4 else (ymT, ymTb)
                        mm(pt[:], src[:, c % 4, i * 128:(i + 1) * 128], wout[:, c, hf * 512:(hf + 1) * 512], c == 0, c == 7, [srcb, woutb], pb)
                    S.op("dve", lambda e: e.tensor_tensor(out=xres[:, i, hf * 512:(hf + 1) * 512], in0=pt[:], in1=xres[:, i, hf * 512:(hf + 1) * 512], op=ALU.add),
                         reads=[pb, xrb[i]], writes=[xrb[i]])
            stop_here("O", xres[:, 0:4, 0:512], xrb[0])
            S.barrier()
            po_.close()

            ph2 = newstack()
            gb2, gb2b = sbp(ph2, "gb2", [128, D], F32)
            hb2 = [sbp(ph2, "hb2_%d" % i, [128, D], BF16) for i in range(2)]
            sq2, sq2b = sbp(ph2, "sq2", [128, D], BF16)
            st2 = [sbp(ph2, "st2_%d" % i, [128, 4], F32) for i in range(4)]
            cnt2 = [0]

            def norm_T(x_ap, x_b, g_t, g_b, dstT, dstTb, col0, nfeat=D, want_h=False, h_dst=None):
                i = cnt2[0]
                cnt2[0] += 1
                h_t, h_b = hb2[i % 2]
                h_ap = h_t[:]
                if h_dst is not None:
                    h_ap, h_b = h_dst
                s_t, s_b = st2[i % 4]
                S.op("act", lambda e: e.activation(out=sq2[:], in_=x_ap, func=AF.Square, accum_out=s_t[:, 0:1]), reads=[x_b], writes=[sq2b, s_b])
                S.op("act", lambda e: e.activation(out=s_t[:, 1:2], in_=s_t[:, 0:1], func=AF.Sqrt, bias=eps_c[:], scale=1.0 / nfeat),
                     reads=[s_b, eps_cb], writes=[s_b])
                S.op("dve", lambda e: e.reciprocal(out=s_t[:, 2:3], in_=s_t[:, 1:2]), reads=[s_b], writes=[s_b])
                S.op("dve", lambda e: e.scalar_tensor_tensor(out=h_ap, in0=x_ap, scalar=s_t[:, 2:3], in1=g_t[:], op0=ALU.mult, op1=ALU.mult),
                     reads=[x_b, s_b, g_b], writes=[h_b])
                if dstT is None:
                    return h_t, h_b, s_t, s_b
                pt, pb = PS[i % 2]
                ptb = pt[:].bitcast(BF16)
                for c in range(8):
                    S.op("pe", lambda e: e.transpose(ptb[:, c * 128:(c + 1) * 128], h_ap[:, c * 128:(c + 1) * 128], ident[:]), reads=[h_b, identb], writes=[pb])
                S.op("act", lambda e: e.copy(out=dstT[:, :, col0:col0 + 128], in_=ptb.rearrange("p (c t) -> p c t", c=8)), reads=[pb], writes=[dstTb])
                if want_h:
                    return h_t, h_b, s_t, s_b

            def load_w(ph, name, src, kch, n):
                t, b = sbp(ph, name, [128, kch, n], BF16)
                for c in range(kch):
                    S.dma("pool", t[:, c, :], src[c * 128:(c + 1) * 128, :], writes=[b])
                return t, b

            px = newstack()
            S.dma("sp", gb2[:], g_mem.partition_broadcast(128), writes=[gb2b])
            memT, memTb = sbp(px, "memT", [128, 8, 256], BF16)
            mt, mtb = sbp(px, "mt", [128, D], F32)
            for i in range(2):
                S.dma("sp", mt[:], mem[i * 128:(i + 1) * 128, :], writes=[mtb])
                norm_T(mt[:], mtb, gb2, gb2b, memT, memTb, i * 128)
            wk_, wkb_ = load_w(px, "xwk", xw_k, 8, D)
            KmT, KmTb = sbp(px, "KmT", [128, 8, 256], BF16)
            for cc in range(8):
                pt, pb = PS[2 + cc % 2]
                for k in range(8):
                    mm(pt[:, 0:256], wk_[:, k, cc * 128:(cc + 1) * 128], memT[:, k, :], k == 0, k == 7, [wkb_, memTb], pb)
                S.op("act", lambda e: e.copy(out=KmT[:, cc, :], in_=pt[:, 0:256]), reads=[pb], writes=[KmTb])
            wv_, wvb_ = wk_, wkb_
            for c in range(8):
                S.dma("pool", wv_[:, c, :], xw_v[c * 128:(c + 1) * 128, :], writes=[wvb_])
            Vm, Vmb = sbp(px, "Vm", [128, 2, D], BF16)
            for mtile in range(2):
                for hf in range(2):
                    pt, pb = PS[2 + hf]
                    for k in range(8):
                        mm(pt[:], memT[:, k, mtile * 128:(mtile + 1) * 128], wv_[:, k, hf * 512:(hf + 1) * 512], k == 0, k == 7, [wvb_, memTb], pb)
                    S.op("act", lambda e: e.copy(out=Vm[:, mtile, hf * 512:(hf + 1) * 512], in_=pt[:]), reads=[pb], writes=[Vmb])
            wq_, wqb_ = wk_, wkb_
            for c in range(8):
                S.dma("pool", wq_[:, c, :], xw_q[c * 128:(c + 1) * 128, :], writes=[wqb_])
            wo_, wob_ = load_w(px, "xwo", xw_o, 8, D)
            S.dma("sp", gb2[:], g_xa.partition_broadcast(128), reads=[], writes=[gb2b])
            h2TL = [sbp(px, "h2T%d" % i, [128, 8, 512], BF16) for i in range(2)]
            QxT, QxTb = sbp(px, "QxT", [128, 8, 512], BF16)
            oT, oTb = sbp(px, "oT", [128, 8, 512], BF16)
            PTx = [sbp(px, "PTx%d" % i, [128, 512], BF16) for i in range(4)]
            rlx, rlxb = sbp(px, "rlx", [128, 512], F32)
            for ti in range(4):
                norm_T(xres[:, ti, :], xrb[ti], gb2, gb2b, h2TL[0][0], h2TL[0][1], ti * 128)
            for st in range(4):
                h2T, h2Tb = h2TL[st % 2]
                for cc in range(8):
                    pt, pb = PS[2 + cc % 2]
                    for k in range(8):
                        mm(pt[:], wq_[:, k, cc * 128:(cc + 1) * 128], h2T[:, k, :], k == 0, k == 7, [wqb_, h2Tb], pb)
                    S.op("act", lambda e: e.copy(out=QxT[:, cc, :], in_=pt[:]), reads=[pb], writes=[QxTb])
                def x_scores(hh):
                    banks = (PS[4], PS[5]) if hh % 2 == 0 else (PS[6], PS[7])
                    for mb in range(2):
                        pt, pb = banks[mb]
                        P_t, P_b = PTx[(hh % 2) * 2 + mb]
                        for dc in range(2):
                            mm(pt[:], KmT[:, 2 * hh + dc, mb * 128:(mb + 1) * 128], QxT[:, 2 * hh + dc, :], dc == 0, dc == 1, [KmTb, QxTb], pb)
                        S.op("act", lambda e: e.activation(out=P_t[:], in_=pt[:], func=AF.Exp, scale=1.0 / 16.0), reads=[pb], writes=[P_b])

                x_scores(0)
                for hh in range(4):
                    if st + 1 < 4:
                        i_n = (st + 1) * 4 + hh
                        norm_T(xres[:, i_n, :], xrb[i_n], gb2, gb2b, h2TL[(st + 1) % 2][0], h2TL[(st + 1) % 2][1], hh * 128)
                    if hh + 1 < 4:
                        x_scores(hh + 1)
                    Pm = [PTx[(hh % 2) * 2 + mb] for mb in range(2)]
                    pl_, plb_ = PS[2]
                    for mb in range(2):
                        mm(pl_[:], ones_bf[:], Pm[mb][0][:], mb == 0, mb == 1, [ones_bfb, Pm[mb][1]], plb_)
                    S.op("dve", lambda e: e.reciprocal(out=rlx[:], in_=pl_[:]), reads=[plb_], writes=[rlxb])
                    for dc in range(2):
                        pt, pb = PS[3]
                        for mb in range(2):
                            mm(pt[:], Vm[:, mb, (2 * hh + dc) * 128:(2 * hh + dc + 1) * 128], Pm[mb][0][:], mb == 0, mb == 1, [Vmb, Pm[mb][1]], pb)
                        S.op("dve", lambda e: e.tensor_tensor(out=oT[:, 2 * hh + dc, :], in0=pt[:], in1=rlx[:], op=ALU.mult), reads=[pb, rlxb], writes=[oTb])
                for ti in range(4):
                    i = st * 4 + ti
                    for hf in range(2):
                        pt, pb = PS[4 + hf]
                        for c in range(8):
                            mm(pt[:], oT[:, c, ti * 128:(ti + 1) * 128], wo_[:, c, hf * 512:(hf + 1) * 512], c == 0, c == 7, [oTb, wob_], pb)
                        S.op("dve", lambda e: e.tensor_tensor(out=xres[:, i, hf * 512:(hf + 1) * 512], in0=pt[:], in1=xres[:, i, hf * 512:(hf + 1) * 512], op=ALU.add),
                             reads=[pb, xrb[i]], writes=[xrb[i]])
            S.barrier()
            px.close()
            stop_here("X", xres[:, 0:4, 0:512], xrb[0])
            TS = MOE_TS
            NT = MOE_NT
            NSUB = TS // 128
            NSLOT = NT * TS
            pe_ = newstack()
            S.dma("sp", gb2[:], g_moe.partition_broadcast(128), writes=[gb2b])
            h3Tt = [sbp(pe_, "h3Tt%d" % i, [128, 8, 128], BF16) for i in range(2)]
            wr, wrb = sbp(pe_, "wr", [128, 8, 36], BF16)
            bb, bbb = sbp(pe_, "bb", [128, 36], F32)
            S.dma("pool", wr[:, :, 0:4], w_group.rearrange("(c p) n -> p c n", p=128), writes=[wrb])
            S.dma("pool", wr[:, :, 4:36], w_expert.rearrange("(c p) n -> p c n", p=128), writes=[wrb])
            S.dma("sp", bb[:, 0:4], b_group.partition_broadcast(128), writes=[bbb])
            S.dma("sp", bb[:, 4:36], b_expert.partition_broadcast(128), writes=[bbb])
            Lst, Lstb = sbp(pe_, "Lst", [128, 128], BF16)
            S.op("pool", lambda e: e.memset(Lst[:], 1.0), writes=[Lstb])
            S.op("pool", lambda e: e.affine_select(out=Lst[:], in_=Lst[:], pattern=[[1, 128]], compare_op=ALU.is_gt, fill=0.0, base=0, channel_multiplier=-1),
                 reads=[Lstb], writes=[Lstb])
            Mall, Mallb = sbp(pe_, "Mall", [128, 16, 32], BF16)
            Moh, Mohb = sbp(pe_, "Moh", [128, 16, 2, 32], F32)
            wts, wtsb = sbp(pe_, "wts", [128, 16, 2], F32)
            idxf, idxfb = sbp(pe_, "idxf", [128, 16, 2], F32)
            idxs, idxsb = sbp(pe_, "idxs", [128, 16, 2], I32)
            rs, rsb = sbp(pe_, "rs", [128, 192], F32)
            lg = rs[:, 0:36]
            gm, ngm, gs_, gw = rs[:, 36:37], rs[:, 37:38], rs[:, 38:39], rs[:, 39:40]
            goh, gex = rs[:, 40:44], rs[:, 44:48]
            t48 = rs[:, 48:80]
            esel, oh1, e2, oh2 = rs[:, 80:88], rs[:, 88:96], rs[:, 96:104], rs[:, 104:112]
            m1, m2, dd, sgm = rs[:, 120:121], rs[:, 121:122], rs[:, 122:123], rs[:, 123:124]
            ptmp = rs[:, 128:160]
            ptmp2 = rs[:, 160:192]
            yscr = nc.dram_tensor("ye_scr", [NSLOT, D], BF16, kind="Internal").ap()

            def R(eng, fn):
                S.op(eng, fn, reads=[rsb], writes=[rsb])

            def h3tile(i):
                return (ymT if i < 8 else ysT)[:, :, :].rearrange("p c t -> p (c t)")[:, (i % 8) * D:(i % 8 + 1) * D]

            h3b = [Buf("h3_%d" % i) for i in range(16)]
            pr1 = newstack()
            LG, LGb = sbp(pr1, "LG", [128, 16, 36], F32)
            for i in range(16):
                hT_t, hT_b = h3Tt[i % 2]
                norm_T(xres[:, i, :], xrb[i], gb2, gb2b, hT_t, hT_b, 0, h_dst=(h3tile(i), h3b[i]))
                pr, prb = PS[2 + i % 2]
                for k in range(8):
                    mm(pr[:, 0:36], hT_t[:, k, :], wr[:, k, :], k == 0, k == 7, [hT_b, wrb], prb)
                S.op("dve", lambda e: e.tensor_tensor(out=LG[:, i, :], in0=pr[:, 0:36], in1=bb[:], op=ALU.add), reads=[prb, bbb], writes=[LGb])
            rb_, rbb = sbp(pr1, "rb_", [128, 16, 64], F32)
            GLv = LG[:, :, 0:4]
            ELv = LG[:, :, 4:36].rearrange("p t (g e) -> p t g e", g=4)
            gmB, gsB, gwB, m1B, m2B, ddB = [rb_[:, :, c] for c in range(6)]
            gohB_, gexB = rb_[:, :, 8:12], rb_[:, :, 12:16]
            eselB, oh1B, e2B, oh2B = rb_[:, :, 16:24], rb_[:, :, 24:32], rb_[:, :, 32:40], rb_[:, :, 40:48]
            t48B, t48Bb = sbp(pr1, "t48B", [128, 16, 4, 8], F32)

            def RB(eng, fn, extra_r=(), extra_w=()):
                S.op(eng, fn, reads=[rbb, LGb] + list(extra_r), writes=[rbb] + list(extra_w))

            def bc4(v):
                return v.unsqueeze(2).broadcast_to([128, 16, 4])

            def bc8(v):
                return v.unsqueeze(2).broadcast_to([128, 16, 8])

            RB("dve", lambda e: e.tensor_reduce(out=gmB, in_=GLv, axis=AX.X, op=ALU.max))
            RB("dve", lambda e: e.tensor_tensor(out=gohB_, in0=GLv, in1=bc4(gmB), op=ALU.is_ge))
            RB("dve", lambda e: e.tensor_tensor(out=gexB, in0=GLv, in1=bc4(gmB), op=ALU.subtract))
            RB("act", lambda e: e.activation(out=gexB, in_=gexB, func=AF.Exp))
            RB("dve", lambda e: e.tensor_reduce(out=gsB, in_=gexB, axis=AX.X, op=ALU.add))
            RB("dve", lambda e: e.reciprocal(out=gwB, in_=gsB))
            S.op("dve", lambda e: e.tensor_tensor(out=t48B[:], in0=ELv, in1=gohB_.unsqueeze(3).broadcast_to([128, 16, 4, 8]), op=ALU.mult),
                 reads=[rbb, LGb], writes=[t48Bb])
            S.op("dve", lambda e: e.tensor_reduce(out=eselB, in_=t48B[:, :, :, :].rearrange("p t g e -> p t e g"), axis=AX.X, op=ALU.add),
                 reads=[t48Bb], writes=[rbb])
            RB("dve", lambda e: e.tensor_reduce(out=m1B, in_=eselB, axis=AX.X, op=ALU.max))
            RB("dve", lambda e: e.tensor_tensor(out=oh1B, in0=eselB, in1=bc8(m1B), op=ALU.is_ge))
            RB("dve", lambda e: e.scalar_tensor_tensor(out=e2B, in0=oh1B, scalar=-1e30, in1=eselB, op0=ALU.mult, op1=ALU.add))
            RB("dve", lambda e: e.tensor_reduce(out=m2B, in_=e2B, axis=AX.X, op=ALU.max))
            RB("dve", lambda e: e.tensor_tensor(out=oh2B, in0=e2B, in1=bc8(m2B), op=ALU.is_ge))
            RB("dve", lambda e: e.tensor_tensor(out=ddB, in0=m2B, in1=m1B, op=ALU.subtract))
            RB("act", lambda e: e.activation(out=ddB, in_=ddB, func=AF.Sigmoid))
            S.op("dve", lambda e: e.tensor_tensor(out=wts[:, :, 1], in0=ddB, in1=gwB, op=ALU.mult), reads=[rbb], writes=[wtsb])
            S.op("dve", lambda e: e.tensor_tensor(out=wts[:, :, 0], in0=gwB, in1=wts[:, :, 1], op=ALU.subtract), reads=[rbb, wtsb], writes=[wtsb])
            for k2, ohB in ((0, oh1B), (1, oh2B)):
                S.op("dve", lambda e: e.tensor_tensor(out=Moh[:, :, k2, :].rearrange("p t (g e) -> p t g e", g=4),
                                                      in0=gohB_.unsqueeze(3).broadcast_to([128, 16, 4, 8]),
                                                      in1=ohB.unsqueeze(2).broadcast_to([128, 16, 4, 8]), op=ALU.mult), reads=[rbb], writes=[Mohb])
            S.op("dve", lambda e: e.tensor_tensor(out=Mall[:, :, :], in0=Moh[:, :, 0, :], in1=Moh[:, :, 1, :], op=ALU.add), reads=[Mohb], writes=[Mallb])
            S.barrier()
            pr1.close()
            pk = [sbp(pe_, "pk%d" % i, [128, 32], F32) for i in range(6)]
            cntf, ntl, incl, soff, ones32, tmpk = [(p[0][:], p[1]) for p in pk]
            pc_, pcb_ = PS[6]
            for i in range(16):
                mm(pc_[:, 0:32], ones_bf[:], Mall[:, i, :], i == 0, i == 15, [ones_bfb, Mallb], pcb_)
            S.op("dve", lambda e: e.tensor_copy(out=cntf[0], in_=pc_[:, 0:32]), reads=[pcb_], writes=[cntf[1]])
            S.op("dve", lambda e: e.tensor_scalar(out=ntl[0], in0=cntf[0], scalar1=0.5, scalar2=None, op0=ALU.is_gt), reads=[cntf[1]], writes=[ntl[1]])
            for th in [TS * q + 0.5 for q in range(1, 2048 // TS)]:
                S.op("dve", lambda e: e.scalar_tensor_tensor(out=ntl[0], in0=cntf[0], scalar=th, in1=ntl[0], op0=ALU.is_gt, op1=ALU.add),
                     reads=[cntf[1], ntl[1]], writes=[ntl[1]])
            S.op("pool", lambda e: e.memset(ones32[0], 1.0), writes=[ones32[1]])
            S.op("dve", lambda e: e.tensor_tensor_scan(out=incl[0], data0=ones32[0], data1=ntl[0], initial=0.0, op0=ALU.mult, op1=ALU.add),
                 reads=[ones32[1], ntl[1]], writes=[incl[1]])
            S.op("dve", lambda e: e.tensor_tensor(out=soff[0], in0=incl[0], in1=ntl[0], op=ALU.subtract), reads=[incl[1], ntl[1]], writes=[soff[1]])
            S.op("dve", lambda e: e.tensor_scalar(out=soff[0], in0=soff[0], scalar1=float(TS), scalar2=None, op0=ALU.mult), reads=[soff[1]], writes=[soff[1]])
            jfi, jfib = sbp(pe_, "jfi", [128, NT], I32)
            jf, jfb = sbp(pe_, "jf", [128, NT], F32)
            pidi, pidib = sbp(pe_, "pidi", [128, 1], I32)
            pidf, pidfb = sbp(pe_, "pidf", [128, 1], F32)
            eidf, eidfb = sbp(pe_, "eidf", [128, NT], F32)
            widx, widxb = sbp(pe_, "widx", [128, NT], I32)
            pcm = newstack()
            cmpT, cmpTb = sbp(pcm, "cmpT", [128, NT, 32], F32)
            S.op("pool", lambda e: e.iota(jfi[:], pattern=[[1, NT]], base=0, channel_multiplier=0), writes=[jfib])
            S.op("dve", lambda e: e.tensor_copy(out=jf[:], in_=jfi[:]), reads=[jfib], writes=[jfb])
            S.op("pool", lambda e: e.iota(pidi[:], pattern=[[0, 1]], base=0, channel_multiplier=1), writes=[pidib])
            S.op("dve", lambda e: e.tensor_copy(out=pidf[:], in_=pidi[:]), reads=[pidib], writes=[pidfb])
            S.op("dve", lambda e: e.tensor_tensor(out=cmpT[:], in0=incl[0].unsqueeze(1).broadcast_to([128, NT, 32]),
                                                  in1=jf[:, :].unsqueeze(2).broadcast_to([128, NT, 32]), op=ALU.is_le), reads=[incl[1], jfb], writes=[cmpTb])
            S.op("dve", lambda e: e.tensor_reduce(out=eidf[:], in_=cmpT[:], axis=AX.X, op=ALU.add), reads=[cmpTb], writes=[eidfb])
            S.op("dve", lambda e: e.tensor_scalar(out=eidf[:], in0=eidf[:], scalar1=32.0, scalar2=128.0, op0=ALU.min, op1=ALU.mult), reads=[eidfb], writes=[eidfb])
            S.op("dve", lambda e: e.tensor_scalar(out=eidf[:], in0=eidf[:], scalar1=pidf[:, 0:1], scalar2=None, op0=ALU.add), reads=[eidfb, pidfb], writes=[eidfb])
            S.op("dve", lambda e: e.tensor_copy(out=widx[:], in_=eidf[:]), reads=[eidfb], writes=[widxb])
            S.barrier()
            pcm.close()
            bc_reg = nc.gpsimd.to_reg(NSLOT - 1)
            bw_reg = nc.gpsimd.to_reg(4095)

            def indirect(out_ap, in_ap, idx_ap, scatter, reads, writes, reg):
                S._deps("pool", reads, writes, is_dma=True)
                k = S.next_dma_sem("pool")
                off = bass.IndirectOffsetOnAxis(ap=idx_ap, axis=0)
                nc.gpsimd.indirect_dma_start(out=out_ap, out_offset=off if scatter else None, in_=in_ap, in_offset=None if scatter else off,
                                             bounds_check=reg, oob_is_err=False).then_inc(S.dsem[k], 16)
                tok = ("d", k, 16 * S.dcnt[k])
                S._mark(tok, reads, writes)
                return tok

            SI = [sbp(pe_, "SI%d" % i, [128, 1], I32) for i in range(4)]
            for tok in zero_toks:
                S._wait("pool", tok)
            si_ = 0
            scat_toks = []
            for i in range(16):
                pp_, ppb_ = PS[4 + i % 2]
                for i2 in range(i):
                    mm(pp_[:, 0:32], ones_bf[:], Mall[:, i2, :], i2 == 0, False, [ones_bfb, Mallb], ppb_)
                mm(pp_[:, 0:32], Lst[:], Mall[:, i, :], i == 0, True, [Lstb, Mallb], ppb_)
                S.op("dve", lambda e: e.tensor_tensor(out=ptmp, in0=pp_[:, 0:32], in1=soff[0], op=ALU.add), reads=[ppb_, soff[1], rsb], writes=[rsb])
                for k2 in range(2):
                    S.op("dve", lambda e: e.tensor_tensor(out=ptmp2, in0=ptmp, in1=Moh[:, i, k2, :], op=ALU.mult), reads=[rsb, Mohb], writes=[rsb])
                    S.op("dve", lambda e: e.tensor_reduce(out=idxf[:, i, k2:k2 + 1], in_=ptmp2, axis=AX.X, op=ALU.add), reads=[rsb], writes=[idxfb])
                S.op("dve", lambda e: e.tensor_copy(out=idxs[:, i, :], in_=idxf[:, i, :]), reads=[idxfb], writes=[idxsb])
                for k2 in range(2):
                    si_t, si_b = SI[si_ % 4]
                    si_ += 1
                    S.op("dve", lambda e: e.tensor_copy(out=si_t[:], in_=idxs[:, i, k2:k2 + 1]), reads=[idxsb], writes=[si_b])
                    scat_toks.append(indirect(xscr[:, :], h3tile(i), si_t[:, :], True, [h3b[i], si_b], [], bc_reg))
            stop_here("R", xres[:, 0:4, 0:512], xrb[0])
            NW = 4
            WG = [sbp(pe_, "WG%d" % i, [128, 8, 256], BF16) for i in range(NW)]
            WU = [sbp(pe_, "WU%d" % i, [128, 8, 256], BF16) for i in range(NW)]
            WD = [sbp(pe_, "WD%d" % i, [128, 2, D], BF16) for i in range(NW)]
            WI = [sbp(pe_, "WI%d" % i, [128, 1], I32) for i in range(NW)]
            XE = [sbp(pe_, "XE%d" % i, [128, D], BF16) for i in range(4)]
            XT = [sbp(pe_, "XT%d" % i, [128, 8, TS], BF16) for i in range(2)]
            YE = [sbp(pe_, "YE%d" % i, [128, D], BF16) for i in range(2)]
            aT2 = [sbp(pe_, "aT2_%d" % i, [128, 2, TS], BF16) for i in range(2)]
            slt = [sbp(pe_, "slt%d" % i, [128, TS], BF16) for i in range(2)]
            ye_toks = []
            evi = 0
            xei = 0
            yei = 0

            def load_w(j):
                wi_t, wi_b = WI[j % NW]
                S.op("dve", lambda e: e.tensor_copy(out=wi_t[:], in_=widx[:, j:j + 1]), reads=[widxb], writes=[wi_b])
                indirect(WG[j % NW][0][:, :, :].rearrange("p c f -> p (c f)"), wbf[0][:, :], wi_t[:, :], False, [wi_b], [WG[j % NW][1]], bw_reg)
                indirect(WU[j % NW][0][:, :, :].rearrange("p c f -> p (c f)"), wbf[1][:, :], wi_t[:, :], False, [wi_b], [WU[j % NW][1]], bw_reg)
                indirect(WD[j % NW][0][:, :, :].rearrange("p c f -> p (c f)"), wbf[2][:, :], wi_t[:, :], False, [wi_b], [WD[j % NW][1]], bw_reg)

            for tok in conv_toks:
                S._wait("pool", tok)
            load_w(0)
            load_w(1)
            load_w(2)
            for tok in scat_toks:
                S._wait("sp", tok)

            def xload(j):
                for s in range(NSUB):
                    xe_t, xe_b = XE[(j % 2) * NSUB + s]
                    S.dma("sp", xe_t[:], xscr[j * TS + s * 128:j * TS + (s + 1) * 128, :], writes=[xe_b])

            def xpose(j):
                xT_t, xT_b = XT[j % 2]
                for s in range(NSUB):
                    xe_t, xe_b = XE[(j % 2) * NSUB + s]
                    pt, pb = PS[s % 2]
                    ptb = pt[:].bitcast(BF16)
                    for k in range(8):
                        S.op("pe", lambda e: e.transpose(ptb[:, k * 128:(k + 1) * 128], xe_t[:, k * 128:(k + 1) * 128], ident[:]), reads=[xe_b, identb], writes=[pb])
                    if s % 2 == 0:
                        S.op("act", lambda e: e.copy(out=xT_t[:, :, s * 128:(s + 1) * 128], in_=ptb.rearrange("p (c t) -> p c t", c=8)), reads=[pb], writes=[xT_b])
                    else:
                        S.op("dve", lambda e: e.tensor_copy(out=xT_t[:, :, s * 128:(s + 1) * 128], in_=ptb.rearrange("p (c t) -> p c t", c=8)), reads=[pb], writes=[xT_b])

            xload(0)
            xpose(0)
            for j in range(NT):
                if j + 3 < NT:
                    load_w(j + 3)
                if j + 1 < NT:
                    xload(j + 1)
                wg_t, wg_b = WG[j % NW]
                wu_t, wu_b = WU[j % NW]
                wd_t, wd_b = WD[j % NW]
                xT_t, xT_b = XT[j % 2]
                a_t, a_b = aT2[j % 2]
                for f in range(2):
                    pg, pgb2 = PS[2 + 2 * f]
                    pu, pub2 = PS[3 + 2 * f]
                    for k in range(8):
                        mm(pg[:, 0:TS], wg_t[:, k, f * 128:(f + 1) * 128], xT_t[:, k, :], k == 0, k == 7, [wg_b, xT_b], pgb2)
                    for k in range(8):
                        mm(pu[:, 0:TS], wu_t[:, k, f * 128:(f + 1) * 128], xT_t[:, k, :], k == 0, k == 7, [wu_b, xT_b], pub2)
                    sl_t, sl_b = slt[f]
                    S.op("act", lambda e: e.activation(out=sl_t[:], in_=pg[:, 0:TS], func=AF.Silu), reads=[pgb2], writes=[sl_b])
                    S.op("dve", lambda e: e.tensor_tensor(out=a_t[:, f, :], in0=pu[:, 0:TS], in1=sl_t[:], op=ALU.mult), reads=[pub2, sl_b], writes=[a_b])
                if j + 1 < NT:
                    xpose(j + 1)
                for s in range(NSUB):
                    ye_t, ye_b = YE[yei % 2]
                    yei += 1
                    for hf in range(2):
                        pd, pdb = PS[6 + evi % 2]
                        for f in range(2):
                            mm(pd[:], a_t[:, f, s * 128:(s + 1) * 128], wd_t[:, f, hf * 512:(hf + 1) * 512], f == 0, f == 1, [a_b, wd_b], pdb)
                        if evi % 2 == 0:
                            S.op("act", lambda e: e.copy(out=ye_t[:, hf * 512:(hf + 1) * 512], in_=pd[:]), reads=[pdb], writes=[ye_b])
                        else:
                            S.op("dve", lambda e: e.tensor_copy(out=ye_t[:, hf * 512:(hf + 1) * 512], in_=pd[:]), reads=[pdb], writes=[ye_b])
                        evi += 1
                    ye_toks.append(S.dma("sp", yscr[j * TS + s * 128:j * TS + (s + 1) * 128, :], ye_t[:], reads=[ye_b]))
            for tok in ye_toks:
                S._wait("pool", tok)
            S.barrier()
            GB = []
            for wl in (WG, WU):
                for i in range(NW):
                    flat_w = wl[i][0][:, :, :].rearrange("p c f -> p (c f)")
                    for hh in range(2):
                        GB.append((flat_w[:, hh * D:(hh + 1) * D], Buf("gbv%d" % len(GB))))
            NGB = 8
            gi_ = 0
            for i in range(16):
                for k2 in range(2):
                    g_t, g_b = GB[gi_ % NGB]
                    gi_ += 1
                    indirect(g_t, yscr[:, :], idxs[:, i, k2:k2 + 1], False, [idxsb], [g_b], bc_reg)
                    S.op("dve", lambda e: e.scalar_tensor_tensor(out=xres[:, i, :], in0=g_t, scalar=wts[:, i, k2:k2 + 1], in1=xres[:, i, :],
                                                                 op0=ALU.mult, op1=ALU.add), reads=[g_b, wtsb, xrb[i]], writes=[xrb[i]])
            S.barrier()
            pe_.close()
            stop_here("E", xres[:, 0:4, 0:512], xrb[0])
            pf = newstack()
            S.dma("sp", gb2[:], g_fin.partition_broadcast(128), writes=[gb2b])
            fo = [sbp(pf, "fo%d" % i, [128, D], F32) for i in range(4)]
            out_toks = []
            for i in range(16):
                f_t, f_b = fo[i % 4]
                s_t, s_b = st2[i % 4]
                S.op("act", lambda e: e.activation(out=sq2[:], in_=xres[:, i, :], func=AF.Square, accum_out=s_t[:, 0:1]), reads=[xrb[i]], writes=[sq2b, s_b])
                S.op("act", lambda e: e.activation(out=s_t[:, 1:2], in_=s_t[:, 0:1], func=AF.Sqrt, bias=eps_c[:], scale=1.0 / D), reads=[s_b, eps_cb], writes=[s_b])
                S.op("dve", lambda e: e.reciprocal(out=s_t[:, 2:3], in_=s_t[:, 1:2]), reads=[s_b], writes=[s_b])
                S.op("dve", lambda e: e.scalar_tensor_tensor(out=f_t[:], in0=xres[:, i, :], scalar=s_t[:, 2:3], in1=gb2[:], op0=ALU.mult, op1=ALU.mult),
                     reads=[xrb[i], s_b, gb2b], writes=[f_b])
                out_toks.append(S.dma("sp", out[i * 128:(i + 1) * 128, :], f_t[:], reads=[f_b]))
            for tok in out_toks:
                S._wait("sp", tok)
            S.barrier()
            for s_ in reversed(STACKS):
                s_.close()

        except StopBuildOuter:
            pass
    return nc


def kernel(**inputs):
    debug = inputs.pop("_debug", None)
    x = np.ascontiguousarray(inputs["x"], dtype=np.float32)
    pos = np.ascontiguousarray(inputs["positions"], dtype=np.int32)
    memv = np.ascontiguousarray(inputs["mem"], dtype=np.float32)
    nc = build_program(debug)
    shared = {}
    for k, v in inputs.items():
        if k in ("x", "mem", "positions"):
            continue
        a = np.ascontiguousarray(v)
        if k == "norm_final_g":
            shared[k] = a.reshape(1024)
        elif k == "moe_w_expert":
            shared[k] = a[0].reshape(1024, 32)
        elif k == "moe_b_expert":
            shared[k] = a[0].reshape(32)
        elif k in ("moe_w_gate", "moe_w_up"):
            shared["wg_l" if k == "moe_w_gate" else "wu_l"] = np.ascontiguousarray(a[0].reshape(32, 8, 128, 256).transpose(0, 2, 1, 3)).reshape(4096, 2048)
        elif k == "moe_w_down":
            shared["wd_l"] = np.ascontiguousarray(a[0].reshape(32, 2, 128, 1024).transpose(0, 2, 1, 3)).reshape(4096, 2048)
        else:
            shared[k] = a[0]
    in_maps = []
    for c in range(8):
        b, half = c // 2, c % 2
        xa = np.zeros((T_ALL, D), np.float32)
        pa = np.zeros((T_ALL,), np.int32)
        vm = np.ones((T_ALL,), np.float32)
        if half == 0:
            xa[T_OWN:] = x[b, :T_OWN]
            pa[T_OWN:] = pos[b, :T_OWN]
            vm[:T_OWN] = 0.0
        else:
            xa[:] = x[b]
            pa[:] = pos[b]
        m = dict(shared)
        m.update({"xa": xa, "posa": pa, "vmask": vm, "mem": memv[b]})
        in_maps.append(m)
    import os as _os
    _n = int(_os.environ.get('KCORES', '8'))
    res = run_bass_kernel_spmd(nc, in_maps[:_n], core_ids=list(range(_n)))
    if debug is not None:
        return [r["dbg"] for r in res.results]
    outp = np.zeros((4, 4096, D), np.float32)
    for c in range(8):
        b, half = c // 2, c % 2
        outp[b, half * T_OWN:(half + 1) * T_OWN] = res.results[c]["out"]
    return outp
```

```python
import math
from contextlib import ExitStack

import numpy as np
import concourse.bass as bass
import concourse.mybir as mybir
from concourse.bass_utils import run_bass_kernel_spmd

F32 = mybir.dt.float32
BF16 = mybir.dt.bfloat16
I32 = mybir.dt.int32
AF = mybir.ActivationFunctionType
ALU = mybir.AluOpType
AX = mybir.AxisListType

D = 1024
T_OWN = 2048
T_ALL = 4096
EPS = 1e-6
MAGIC = 12582912.0
TWO_PI = 2.0 * math.pi
SIN_SCALE = TWO_PI * (1.0 - 2e-6)

NDMA = 32
NDMA_HW = 16


class Buf:
    __slots__ = ("name", "w", "r")

    def __init__(self, name):
        self.name = name
        self.w = []
        self.r = {}


class Sync:
    def __init__(self, nc, stack):
        self.nc = nc
        self.h = {"pe": nc.tensor, "act": nc.scalar, "dve": nc.vector, "pool": nc.gpsimd, "sp": nc.sync}
        self.sem = {k: stack.enter_context(nc.semaphore("s_" + k)) for k in self.h}
        self.cnt = {k: 0 for k in self.h}
        self.seen = {k: {} for k in self.h}
        self.dsem = [stack.enter_context(nc.semaphore("d%d" % i)) for i in range(NDMA)]
        self.dcnt = [0] * NDMA
        self.drr = 0
        self.drr_sw = 0

    def _wait(self, eng, tok):
        kind, key, val = tok
        if kind == "e" and key == eng and eng in ("pe", "sp"):
            return
        k = (kind, key)
        if self.seen[eng].get(k, 0) >= val:
            return
        self.seen[eng][k] = val
        sem = self.sem[key] if kind == "e" else self.dsem[key]
        self.h[eng].wait_ge(sem, val)

    def _deps(self, eng, reads, writes, is_dma=False):
        toks = []
        for b in reads:
            toks.extend(b.w)
        for b in writes:
            for t in b.w:
                if not (is_dma and t[0] == "d"):
                    toks.append(t)
            toks.extend(b.r.values())
        for t in toks:
            self._wait(eng, t)

    def _mark(self, tok, reads, writes):
        for b in reads:
            b.r[(tok[0], tok[1])] = tok
        for b in writes:
            if tok[0] == "d":
                b.w = [t for t in b.w if t[0] == "d"] + [tok]
            else:
                b.w = [tok]
            b.r = {}

    def op(self, eng, fn, reads=(), writes=()):
        self._deps(eng, reads, writes)
        inst = fn(self.h[eng])
        self.cnt[eng] += 1
        inst.then_inc(self.sem[eng], 1)
        tok = ("e", eng, self.cnt[eng])
        self._mark(tok, reads, writes)
        return tok

    def next_dma_sem(self, eng):
        if eng == "pool":
            k = NDMA_HW + self.drr_sw
            self.drr_sw = (self.drr_sw + 1) % (NDMA - NDMA_HW)
        else:
            k = self.drr
            self.drr = (self.drr + 1) % NDMA_HW
        if self.dcnt[k] > 0:
            self._wait(eng, ("d", k, 16 * self.dcnt[k]))
        self.dcnt[k] += 1
        return k

    def dma(self, eng, out, in_, reads=(), writes=(), **kw):
        self._deps(eng, reads, writes, is_dma=True)
        k = self.next_dma_sem(eng)
        self.h[eng].dma_start(out=out, in_=in_, **kw).then_inc(self.dsem[k], 16)
        tok = ("d", k, 16 * self.dcnt[k])
        self._mark(tok, reads, writes)
        return tok

    def barrier(self):
        for eng in self.h:
            for other in self.h:
                if other != eng and self.cnt[other] > 0:
                    self._wait(eng, ("e", other, self.cnt[other]))
            for k in range(NDMA):
                if self.dcnt[k] > 0:
                    self._wait(eng, ("d", k, 16 * self.dcnt[k]))


class StopBuildOuter(Exception):
    pass


def build_program(debug=None):
    nc = bass.Bass("TRN2", target_bir_lowering=False)
    dt_in = {}

    def din(name, shape, dt=F32):
        dt_in[name] = nc.dram_tensor(name, list(shape), dt, kind="ExternalInput").ap()
        return dt_in[name]

    xa = din("xa", [T_ALL, D])
    posa = din("posa", [T_ALL], I32)
    vmask = din("vmask", [T_ALL])
    mem = din("mem", [256, D])
    g_mix = din("norm_mix_g", [D])
    w_in = din("w_in", [D, 1568])
    lam_re = din("ssm_lam_re", [32, 64])
    lam_im = din("ssm_lam_im", [32, 64])
    log_dt = din("ssm_log_dt", [32])
    b_re = din("ssm_b_re", [32, 64, 16])
    b_im = din("ssm_b_im", [32, 64, 16])
    c_re = din("ssm_c_re", [32, 16, 64])
    c_im = din("ssm_c_im", [32, 16, 64])
    ssm_d = din("ssm_d", [512])
    w_glu = din("ssm_w_glu", [512, 512])
    b_glu = din("ssm_b_glu", [512])
    g_q = din("mla_q_norm_g", [768])
    w_qup = din("mla_w_q_up", [768, 768])
    g_kv = din("mla_kv_norm_g", [256])
    w_kvup = din("mla_w_kv_up", [256, 1024])
    g_os = din("out_norm_ssm_g", [512])
    g_om = din("out_norm_mla_g", [512])
    w_out = din("w_out", [D, D])
    g_xa = din("norm_xattn_g", [D])
    g_mem = din("norm_mem_g", [D])
    xw_q = din("xattn_w_q", [D, D])
    xw_k = din("xattn_w_k", [D, D])
    xw_v = din("xattn_w_v", [D, D])
    xw_o = din("xattn_w_o", [D, D])
    g_moe = din("norm_moe_g", [D])
    w_group = din("moe_w_group", [D, 4])
    b_group = din("moe_b_group", [4])
    w_expert = din("moe_w_expert", [D, 32])
    b_expert = din("moe_b_expert", [32])
    wg_l = din("wg_l", [4096, 2048])
    wu_l = din("wu_l", [4096, 2048])
    wd_l = din("wd_l", [4096, 2048])
    g_fin = din("norm_final_g", [D])
    out = nc.dram_tensor("out", [T_OWN, D], F32, kind="ExternalOutput").ap()
    dbg = None
    if debug is not None:
        dbg = nc.dram_tensor("dbg", list(debug), F32, kind="ExternalOutput").ap()

    es = ExitStack()
    with es:
        try:
            S = Sync(nc, es)
            es.enter_context(nc.allow_low_precision("bf16 matmul operand staging; fp32 accumulation"))

            def sb(name, shape, dt=F32):
                t = es.enter_context(nc.sbuf_tensor(name, list(shape), dt))
                return t, Buf(name)

            PS = []
            for i in range(8):
                t = es.enter_context(nc.psum_tensor("ps%d" % i, [128, 512], F32))
                PS.append((t, Buf("ps%d" % i)))

            def mm(ps_ap, lhsT, rhs, start, stop, reads, wbuf):
                S.op("pe", lambda e: e.matmul(ps_ap, lhsT, rhs, start=start, stop=stop), reads=reads, writes=[wbuf])

            ident_f, ident_fb = sb("ident_f", [128, 128], F32)
            ident, identb = sb("ident", [128, 128], BF16)
            ones_bf, ones_bfb = sb("ones_bf", [128, 128], BF16)
            S.op("pool", lambda e: e.memset(ident_f[:], 1.0), writes=[ident_fb])
            S.op("pool", lambda e: e.affine_select(out=ident_f[:], in_=ident_f[:], pattern=[[-1, 128]],
                                                   compare_op=ALU.is_equal, fill=0.0, base=0, channel_multiplier=1),
                 reads=[ident_fb], writes=[ident_fb])
            S.op("dve", lambda e: e.tensor_copy(out=ident[:], in_=ident_f[:]), reads=[ident_fb], writes=[identb])
            S.op("pool", lambda e: e.memset(ones_bf[:], 1.0), writes=[ones_bfb])

            def bcast_load(name, src, n):
                t, b = sb(name, [128, n], F32)
                S.dma("sp", t[:], src.partition_broadcast(128), writes=[b])
                return t, b

            gmix_b, gmix_bb = bcast_load("gmix_b", g_mix, D)

            def col_load(name, src, nchunk):
                t, b = sb(name, [128, nchunk], F32)
                with nc.allow_non_contiguous_dma("small param column load"):
                    S.dma("sp", t[:], src.rearrange("(c p) -> p c", p=128), writes=[b])
                return t, b

            STACKS = []

            def newstack():
                s_ = ExitStack()
                STACKS.append(s_)
                return s_

            class StopBuild(StopBuildOuter):
                pass

            def stop_here(tag, var=None, varb=None):
                import os
                if os.environ.get("KSTOP", "") != tag:
                    return
                S.barrier()
                if var is not None and debug is not None:
                    dst_ = newstack()
                    dtmp, dtmpb = sbp(dst_, "dtmp_" + tag, [128, 4, 512], F32)
                    S.op("dve", lambda e: e.tensor_copy(out=dtmp[:], in_=var), reads=[varb], writes=[dtmpb])
                    tok = S.dma("sp", dbg.rearrange("(c p) t -> p c t", p=128), dtmp[:], reads=[dtmpb])
                    S._wait("sp", tok)
                S.barrier()
                for s_ in reversed(STACKS):
                    s_.close()
                raise StopBuild()

            def sbp(ph, name, shape, dt=F32):
                t = ph.enter_context(nc.sbuf_tensor(name, list(shape), dt))
                return t, Buf(name)

            def rsqrt_to(out_ap, outb, in_ap, inb, scale, tmp_ap, tmpb, npart=128):
                S.op("act", lambda e: e.activation(out=tmp_ap, in_=in_ap, func=AF.Sqrt, bias=eps_c[0:npart, :], scale=scale),
                     reads=[inb, eps_cb], writes=[tmpb])
                S.op("dve", lambda e: e.reciprocal(out=out_ap, in_=tmp_ap), reads=[tmpb], writes=[outb])

            eps_c, eps_cb = sb("eps_c", [128, 1], F32)
            sinb_c, sinb_cb = sb("sinb_c", [128, 2], F32)
            S.op("pool", lambda e: e.memset(sinb_c[:, 0:1], 0.0), writes=[sinb_cb])
            S.op("pool", lambda e: e.memset(sinb_c[:, 1:2], 0.25 * SIN_SCALE), reads=[sinb_cb], writes=[sinb_cb])
            S.op("pool", lambda e: e.memset(eps_c[:], EPS), writes=[eps_cb])
            MOE_TS = 256
            MOE_NT = 32 + 4096 // MOE_TS
            xscr = nc.dram_tensor("xe_scr", [MOE_NT * MOE_TS, D], BF16, kind="Internal").ap()
            ztile, ztileb = sb("ztile", [128, D], BF16)
            S.op("pool", lambda e: e.memset(ztile[:], 0.0), writes=[ztileb])
            swapm, swapmb = sb("swapm", [128, 128], BF16)
            S.op("dve", lambda e: e.tensor_copy(out=swapm[:, 0:64], in_=ident_f[:, 64:128]), reads=[ident_fb], writes=[swapmb])
            S.op("dve", lambda e: e.tensor_copy(out=swapm[:, 64:128], in_=ident_f[:, 0:64]), reads=[ident_fb], writes=[swapmb])

            stackY = newstack()
            ymT, ymTb = sbp(stackY, "ymT", [128, 4, T_OWN], BF16)
            ysT, ysTb = sbp(stackY, "ysT", [128, 4, T_OWN], BF16)
            sqn, sqnb = sbp(stackY, "sqn", [128, 6, 512], BF16)
            rtmp, rtmpb = sbp(stackY, "rtmp", [128, 512], F32)
            stackU = newstack()
            uT, uTb = sbp(stackU, "uT", [128, 4, 8, T_ALL // 8], BF16)

            pam = newstack()
            ckvT, ckvTb = sbp(pam, "ckvT", [128, 2, T_ALL], BF16)
            cqT, cqTb = sbp(pam, "cqT", [128, 6, T_OWN], BF16)
            rkvb, rkvbb = sbp(pam, "rkvb", [128, T_ALL], BF16)
            rkvt, rkvtb = sbp(pam, "rkvt", [128, 32], F32)
            rqb, rqbb = sbp(pam, "rqb", [128, T_OWN], BF16)
            cs, csb = sbp(pam, "cs", [128, T_ALL], BF16)
            kpeb = Buf("kpe")
            vmt, vmtb = sbp(pam, "vmt", [128, 32], F32)
            gkv_c, gkv_cb = sbp(pam, "gkv_c", [128, 2], F32)
            gq_c, gq_cb = sbp(pam, "gq_c", [128, 6], F32)
            with nc.allow_non_contiguous_dma("small param loads"):
                S.dma("sp", vmt[:], vmask.rearrange("(t p) -> p t", p=128), writes=[vmtb])
                S.dma("sp", gkv_c[:], g_kv.rearrange("(c p) -> p c", p=128), writes=[gkv_cb])
                S.dma("sp", gq_c[:], g_q.rearrange("(c p) -> p c", p=128), writes=[gq_cb])

            pa = newstack()
            win, winb = sbp(pa, "win", [128, 8, 1600], BF16)
            for c in range(8):
                S.dma("pool", win[:, c, 0:1568], w_in[c * 128:(c + 1) * 128, :], writes=[winb])
            S.op("dve", lambda e: e.tensor_scalar(out=win[:, :, 1568:1584], in0=win[:, :, 1552:1568], scalar1=-1.0,
                                                  scalar2=None, op0=ALU.mult), reads=[winb], writes=[winb])
            S.op("dve", lambda e: e.tensor_copy(out=win[:, :, 1584:1600], in_=win[:, :, 1536:1552]), reads=[winb], writes=[winb])

            pcs = newstack()
            ri, rib = sbp(pcs, "ri", [64, 1], I32)
            rf, rfb = sbp(pcs, "rf", [64, 4], F32)
            S.op("pool", lambda e: e.iota(ri[:], pattern=[[0, 1]], base=0, channel_multiplier=1), writes=[rib])
            S.op("dve", lambda e: e.tensor_single_scalar(out=ri[:], in_=ri[:], scalar=15, op=ALU.bitwise_and), reads=[rib], writes=[rib])
            S.op("dve", lambda e: e.tensor_copy(out=rf[:, 0:1], in_=ri[:]), reads=[rib], writes=[rfb])
            S.op("act", lambda e: e.activation(out=rf[:, 1:2], in_=rf[:, 0:1], func=AF.Exp, scale=-math.log(10000.0) / 16.0),
                 reads=[rfb], writes=[rfb])
            S.op("dve", lambda e: e.tensor_scalar(out=rf[:, 2:3], in0=rf[:, 1:2], scalar1=1.0 / TWO_PI, scalar2=None, op0=ALU.mult),
                 reads=[rfb], writes=[rfb])
            S.op("pool", lambda e: e.memset(rf[0:32, 3:4], 0.25), writes=[rfb])
            S.op("pool", lambda e: e.memset(rf[32:64, 3:4], 0.0), writes=[rfb])
            posi, posib = sbp(pcs, "posi", [64, 512], I32)
            ya, yab = sbp(pcs, "ya", [64, 512], F32)
            yb_, ybb = sbp(pcs, "yb_", [64, 512], F32)
            yc_, ycb = sbp(pcs, "yc_", [64, 512], F32)

            def range_reduce_sin(out_ap, outb, y_ap, yb, t_ap, tb, n_ap, nb, eng="dve", pre=0.0):
                if pre == 0.0:
                    S.op(eng, lambda e: e.tensor_scalar(out=t_ap, in0=y_ap, scalar1=MAGIC, scalar2=None, op0=ALU.add), reads=[yb], writes=[tb])
                else:
                    S.op(eng, lambda e: e.tensor_scalar(out=t_ap, in0=y_ap, scalar1=pre, scalar2=MAGIC, op0=ALU.add, op1=ALU.add), reads=[yb], writes=[tb])
                S.op(eng, lambda e: e.scalar_tensor_tensor(out=n_ap, in0=t_ap, scalar=-MAGIC, in1=y_ap, op0=ALU.add, op1=ALU.subtract), reads=[tb, yb], writes=[nb])
                S.op("act", lambda e: e.activation(out=out_ap, in_=n_ap, func=AF.Sin, scale=-SIN_SCALE, bias=(sinb_c[0:out_ap.shape[0], 0:1] if pre == 0.0 else sinb_c[0:out_ap.shape[0], 1:2])),
                     reads=[nb, sinb_cb], writes=[outb])

            for ch in range(8):
                S.dma("sp", posi[:], posa[ch * 512:(ch + 1) * 512].partition_broadcast(64), writes=[posib])
                S.op("dve", lambda e: e.tensor_copy(out=ya[:], in_=posi[:]), reads=[posib], writes=[yab])
                S.op("dve", lambda e: e.tensor_scalar(out=ya[:], in0=ya[:], scalar1=rf[:, 2:3], scalar2=rf[:, 3:4], op0=ALU.mult, op1=ALU.add),
                     reads=[yab, rfb], writes=[yab])
                range_reduce_sin(cs[0:64, ch * 512:(ch + 1) * 512], csb, ya[:], yab, yb_[:], ybb, yc_[:], ycb)

            S.barrier()
            pcs.close()
            xt = [sbp(pa, "xt%d" % i, [128, D], F32) for i in range(2)]
            hb = [sbp(pa, "hb%d" % i, [128, D], BF16) for i in range(2)]
            hT = [sbp(pa, "hT%d" % i, [128, 8, 512], BF16) for i in range(2)]
            sq_junk, sq_junkb = sbp(pa, "sq_junk", [128, D], BF16)
            stat = [sbp(pa, "stat%d" % i, [128, 4], F32) for i in range(4)]
            ropeA, ropeAb = sbp(pa, "ropeA", [32, 512], F32)
            ropeB, ropeBb = sbp(pa, "ropeB", [32, 512], F32)
            sst, sstb = sbp(pa, "sst", [128, 4], F32)

            def rms_rows(x_t, x_b, s_t, s_b, nfeat):
                S.op("act", lambda e: e.activation(out=sq_junk[:], in_=x_t, func=AF.Square, accum_out=s_t[:, 0:1]),
                     reads=[x_b], writes=[sq_junkb, s_b])
                S.op("act", lambda e: e.activation(out=s_t[:, 1:2], in_=s_t[:, 0:1], func=AF.Sqrt, bias=eps_c[:], scale=1.0 / nfeat),
                     reads=[s_b, eps_cb], writes=[s_b])
                S.op("dve", lambda e: e.reciprocal(out=s_t[:, 2:3], in_=s_t[:, 1:2]), reads=[s_b], writes=[s_b])

            zi = 0

            def a_stage1(tile_idx):
                x_t, x_b = xt[tile_idx % 2]
                h_t, h_b = hb[tile_idx % 2]
                s_t, s_b = stat[tile_idx % 4]
                S.dma("sp", x_t[:], xa[tile_idx * 128:(tile_idx + 1) * 128, :], writes=[x_b])
                rms_rows(x_t[:], x_b, s_t, s_b, D)
                S.op("dve", lambda e: e.scalar_tensor_tensor(out=h_t[:], in0=x_t[:], scalar=s_t[:, 2:3], in1=gmix_b[:],
                                                             op0=ALU.mult, op1=ALU.mult),
                     reads=[x_b, s_b, gmix_bb], writes=[h_b])

            def a_stage2(tile_idx, hTt, hTb, ti):
                h_t, h_b = hb[tile_idx % 2]
                pt, pb = PS[tile_idx % 2]
                ptb = pt[:].bitcast(BF16)
                for c in range(8):
                    S.op("pe", lambda e: e.transpose(ptb[:, c * 128:(c + 1) * 128], h_t[:, c * 128:(c + 1) * 128], ident[:]),
                         reads=[h_b, identb], writes=[pb])
                S.op("dve" if tile_idx % 2 else "act", (lambda e: e.tensor_copy(out=hTt[:, :, ti * 128:(ti + 1) * 128], in_=ptb.rearrange("p (c t) -> p c t", c=8))) if tile_idx % 2 else
                     (lambda e: e.copy(out=hTt[:, :, ti * 128:(ti + 1) * 128], in_=ptb.rearrange("p (c t) -> p c t", c=8))),
                     reads=[pb], writes=[hTb])

            a_stage1(0)
            for ti in range(4):
                a_stage1(ti + 1)
                a_stage2(ti, hT[0][0], hT[0][1], ti)
            for st in range(8):
                hTt, hTb = hT[st % 2]
                cols = slice(st * 512, (st + 1) * 512)
                pend_tiles = list(range(4)) if st + 1 < 8 else []

                def next_tile():
                    if pend_tiles:
                        ti = pend_tiles.pop(0)
                        tile_idx = (st + 1) * 4 + ti
                        if tile_idx + 1 < 32:
                            a_stage1(tile_idx + 1)
                        a_stage2(tile_idx, hT[(st + 1) % 2][0], hT[(st + 1) % 2][1], ti)

                def zchunk(c0, m):
                    nonlocal zi
                    pt, pb = PS[2 + (zi % 2)]
                    zi += 1
                    for k in range(8):
                        mm(pt[0:m, :], win[:, k, c0:c0 + m], hTt[:, k, :], k == 0, k == 7, [winb, hTb], pb)
                    return pt, pb

                for c in range(4):
                    pt, pb = zchunk(c * 128, 128)
                    S.op("act", lambda e: e.copy(out=uT[:, c, :, st * 64:(st + 1) * 64], in_=pt[:].rearrange("p (m j) -> p j m", j=8)), reads=[pb], writes=[uTb])
                    if c % 2 == 1:
                        next_tile()
                for c in range(2):
                    pt, pb = zchunk(1280 + c * 128, 128)
                    S.op("act", lambda e: e.activation(out=ckvT[:, c, cols], in_=pt[:], func=AF.Copy, scale=gkv_c[:, c:c + 1]),
                         reads=[pb, gkv_cb], writes=[ckvTb])
                    S.op("act", lambda e: e.activation(out=sqn[:, c, :], in_=pt[:], func=AF.Square), reads=[pb], writes=[sqnb])
                pss, pssb = PS[4]
                for c in range(2):
                    mm(pss[:], ones_bf[:], sqn[:, c, :], c == 0, c == 1, [ones_bfb, sqnb], pssb)
                rsqrt_to(rkvb[:, cols], rkvbb, pss[:], pssb, 1.0 / 256, rtmp[:], rtmpb)
                pst, pstb = PS[6]
                for ti in range(4):
                    for c in range(2):
                        mm(pst[:, ti:ti + 1], sqn[:, c, ti * 128:(ti + 1) * 128], ones_bf[:, 0:1], c == 0, c == 1, [ones_bfb, sqnb], pstb)
                rsqrt_to(rkvt[:, st * 4:(st + 1) * 4], rkvtb, pst[:, 0:4], pstb, 1.0 / 256, sst[:], sstb)
                next_tile()
                pt, pb = zchunk(1536, 64)
                S.op("dve", lambda e: e.tensor_tensor(out=ropeA[:], in0=pt[0:32, :], in1=cs[0:32, cols], op=ALU.mult), reads=[pb, csb], writes=[ropeAb])
                S.op("dve", lambda e: e.tensor_tensor(out=ropeB[:], in0=pt[32:64, :], in1=cs[32:64, cols], op=ALU.mult), reads=[pb, csb], writes=[ropeBb])
                S.op("dve", lambda e: e.tensor_tensor(out=cs[64:96, cols], in0=ropeA[:], in1=ropeB[:], op=ALU.add), reads=[ropeAb, ropeBb], writes=[kpeb])
                if st >= 4:
                    ocols = slice((st - 4) * 512, (st - 3) * 512)
                    for c in range(6):
                        pt, pb = zchunk(512 + c * 128, 128)
                        S.op("act", lambda e: e.activation(out=cqT[:, c, ocols], in_=pt[:], func=AF.Copy, scale=gq_c[:, c:c + 1]),
                             reads=[pb, gq_cb], writes=[cqTb])
                        S.op("act", lambda e: e.activation(out=sqn[:, c, :], in_=pt[:], func=AF.Square), reads=[pb], writes=[sqnb])
                    for c in range(6):
                        mm(pss[:], ones_bf[:], sqn[:, c, :], c == 0, c == 5, [ones_bfb, sqnb], pssb)
                    rsqrt_to(rqb[:, ocols], rqbb, pss[:], pssb, 1.0 / 768, rtmp[:], rtmpb)
                while pend_tiles:
                    next_tile()
            stop_here("A")
            S.barrier()
            pa.close()
            pm = newstack()
            wkv, wkvb = sbp(pm, "wkv", [128, 2, 8, 128], BF16)
            for c in range(2):
                S.dma("pool", wkv[:, c, :, :].rearrange("p h d -> p (h d)"), w_kvup[c * 128:(c + 1) * 128, :], writes=[wkvb])
            wq, wqb = sbp(pm, "wq", [128, 6, 8, 128], BF16)
            pw_ = newstack()
            wqs, wqsb = sbp(pw_, "wqs", [128, 6, 8, 96], BF16)
            for c in range(6):
                S.dma("pool", wqs[:, c, :, :].rearrange("p h d -> p (h d)"), w_qup[c * 128:(c + 1) * 128, :], writes=[wqsb])
            S.op("dve", lambda e: e.tensor_copy(out=wq[:, :, :, 0:32], in_=wqs[:, :, :, 64:96]), reads=[wqsb], writes=[wqb])
            S.op("dve", lambda e: e.tensor_scalar(out=wq[:, :, :, 32:48], in0=wqs[:, :, :, 80:96], scalar1=-1.0, scalar2=None, op0=ALU.mult),
                 reads=[wqsb], writes=[wqb])
            S.op("dve", lambda e: e.tensor_copy(out=wq[:, :, :, 48:64], in_=wqs[:, :, :, 64:80]), reads=[wqsb], writes=[wqb])
            S.op("dve", lambda e: e.tensor_copy(out=wq[:, :, :, 64:128], in_=wqs[:, :, :, 0:64]), reads=[wqsb], writes=[wqb])
            S.barrier()
            pw_.close()
            KT = [sbp(pm, "KT%d" % i, [128, T_ALL], BF16) for i in range(2)]
            VA = [sbp(pm, "VA%d" % i, [128, 32, 128], BF16) for i in range(2)]
            QT = [sbp(pm, "QT%d" % i, [128, T_OWN], BF16) for i in range(2)]
            PT = [sbp(pm, "PT%d" % i, [128, 512], BF16) for i in range(4)]
            SCB = [PS[0], PS[1], PS[7]]
            qA, qAb = sbp(pm, "qA", [32, 512], F32)
            qB, qBb = sbp(pm, "qB", [32, 512], F32)
            rl, rlb = sbp(pm, "rl", [64, 512], F32)
            S.op("dve", lambda e: e.tensor_tensor(out=cs[0:64, T_OWN:T_ALL], in0=cs[0:64, T_OWN:T_ALL], in1=rqb[0:64, :], op=ALU.mult), reads=[csb, rqbb], writes=[csb])
            csq, csqb = cs[:, T_OWN:T_ALL], csb
            for i in range(2):
                S.op("pool", lambda e: e.memset(KT[i][0][32:64, :], 0.0), writes=[KT[i][1]])
                S.op("pool", lambda e: e.memset(QT[i][0][32:64, :], 0.0), writes=[QT[i][1]])
                S.op("dve", lambda e: e.tensor_copy(out=VA[i][0][:, :, 64:128], in_=vmt[:, :].unsqueeze(2).broadcast_to([128, 32, 64])),
                     reads=[vmtb], writes=[VA[i][1]])
            zero_toks = []
            for r0 in range(0, MOE_NT * MOE_TS, 1024):
                zero_toks.append(S.dma("sp", xscr[r0:r0 + 1024, :].rearrange("(s p) d -> p s d", p=128),
                                       ztile[:, :].unsqueeze(1).broadcast_to([128, 8, D]), reads=[ztileb]))
            wbf = [nc.dram_tensor("wbf%d" % i, [4096, 2048], BF16, kind="Internal").ap() for i in range(3)]
            conv_toks = []
            for r0 in range(0, 4096, 512):
                for i, src in enumerate((wg_l, wu_l, wd_l)):
                    conv_toks.append(S.dma("pool", wbf[i][r0:r0 + 512, :], src[r0:r0 + 512, :]))
            SCALE = 96.0 ** -0.5
            pti = 0
            psi = 0
            def build_K(h):
                KTt, KTb = KT[h % 2]
                VAt, VAb = VA[h % 2]
                QTt, QTb = QT[h % 2]
                S.op("act", lambda e: e.copy(out=KTt[0:32, :], in_=cs[64:96, :]), reads=[kpeb], writes=[KTb])
                for st in range(8):
                    cols = slice(st * 512, (st + 1) * 512)
                    pk, pkb = PS[4] if st % 2 == 0 else PS[6]
                    for c in range(2):
                        mm(pk[:, :], wkv[:, c, h, :], ckvT[:, c, cols], c == 0, c == 1, [wkvb, ckvTb], pkb)
                    S.op("dve", lambda e: e.tensor_tensor(out=KTt[64:128, cols], in0=pk[0:64, :], in1=rkvb[0:64, cols], op=ALU.mult),
                         reads=[pkb, rkvbb], writes=[KTb])

            def build_V(h):
                KTt, KTb = KT[h % 2]
                VAt, VAb = VA[h % 2]
                QTt, QTb = QT[h % 2]
                for kg in range(4):
                    pv, pvb = PS[5] if kg % 2 == 0 else PS[4]
                    for j in range(8):
                        kt = kg * 8 + j
                        for c in range(2):
                            mm(pv[:, j * 64:(j + 1) * 64], ckvT[:, c, kt * 128:(kt + 1) * 128], wkv[:, c, h, 64:128], c == 0, c == 1,
                               [wkvb, ckvTb], pvb)
                    S.op("dve", lambda e: e.tensor_tensor(out=VAt[:, kg * 8:(kg + 1) * 8, 0:64],
                                                          in0=pv[:].rearrange("p (j d) -> p j d", j=8),
                                                          in1=rkvt[:, kg * 8:(kg + 1) * 8].unsqueeze(2).broadcast_to([128, 8, 64]), op=ALU.mult),
                         reads=[pvb, rkvtb], writes=[VAb])

            def build_Q(h):
                KTt, KTb = KT[h % 2]
                VAt, VAb = VA[h % 2]
                QTt, QTb = QT[h % 2]
                for st in range(4):
                    cols = slice(st * 512, (st + 1) * 512)
                    pq, pqb = PS[6] if st % 2 == 0 else PS[4]
                    for c in range(6):
                        mm(pq[:], wq[:, c, h, :], cqT[:, c, cols], c == 0, c == 5, [wqb, cqTb], pqb)
                    S.op("dve", lambda e: e.tensor_tensor(out=QTt[64:128, cols], in0=pq[64:128, :], in1=rqb[64:128, cols], op=ALU.mult),
                         reads=[pqb, rqbb], writes=[QTb])
                    S.op("dve", lambda e: e.tensor_tensor(out=qA[:], in0=pq[0:32, :], in1=csq[0:32, cols], op=ALU.mult), reads=[pqb, csqb], writes=[qAb])
                    S.op("dve", lambda e: e.tensor_tensor(out=qB[:], in0=pq[32:64, :], in1=csq[32:64, cols], op=ALU.mult), reads=[pqb, csqb], writes=[qBb])
                    S.op("dve", lambda e: e.tensor_tensor(out=QTt[0:32, cols], in0=qA[:], in1=qB[:], op=ALU.add), reads=[qAb, qBb], writes=[QTb])


            def build_head(h):
                build_K(h)
                build_V(h)
                build_Q(h)

            def attn_head(h, st):
                nonlocal psi, pti
                KTt, KTb = KT[h % 2]
                VAt, VAb = VA[h % 2]
                QTt, QTb = QT[h % 2]
                po, pob = PS[2 + ((h * 4 + st) % 2)]
                nkb = 16 + 4 * st + 4
                pend = []
                for kb in range(nkb):
                    j = kb - (16 + 4 * st)
                    c0 = 128 * j if j > 0 else 0
                    pss_, pssb_ = SCB[psi % 3]
                    psi += 1
                    P_t, P_b = PT[pti % 4]
                    pti += 1
                    mm(pss_[:, c0:512], KTt[:, kb * 128:(kb + 1) * 128], QTt[:, st * 512 + c0:(st + 1) * 512], True, True, [KTb, QTb], pssb_)
                    S.op("act", lambda e: e.activation(out=P_t[:, c0:512], in_=pss_[:, c0:512], func=AF.Exp, scale=SCALE), reads=[pssb_], writes=[P_b])
                    if j >= 0:
                        S.op("dve", lambda e: e.memset(P_t[64:128, c0:c0 + 64], 0.0), writes=[P_b])
                    pend.append((kb, c0, P_t, P_b))
                    if len(pend) > 2:
                        pkb, pc0, pP_t, pP_b = pend.pop(0)
                        mm(po[:, pc0:512], VAt[:, pkb, :], pP_t[:, pc0:512], pkb == 0, False, [VAb, pP_b], pob)
                while pend:
                    pkb, pc0, pP_t, pP_b = pend.pop(0)
                    mm(po[:, pc0:512], VAt[:, pkb, :], pP_t[:, pc0:512], pkb == 0, len(pend) == 0, [VAb, pP_b], pob)
                S.op("dve", lambda e: e.reciprocal(out=rl[:], in_=po[64:128, :]), reads=[pob], writes=[rlb])
                r0 = (h % 2) * 64
                S.op("dve", lambda e: e.tensor_tensor(out=ymT[r0:r0 + 64, h // 2, st * 512:(st + 1) * 512], in0=po[0:64, :], in1=rl[:], op=ALU.mult),
                     reads=[pob, rlb], writes=[ymTb])

            build_head(0)
            for h in range(8):
                for st in range(4):
                    attn_head(h, st)
                    if st == 1 and h + 1 < 8:
                        build_head(h + 1)
            gom_c, gom_cb = sbp(pm, "gom_c", [128, 4], F32)
            with nc.allow_non_contiguous_dma("small param loads"):
                S.dma("sp", gom_c[:], g_om.rearrange("(c p) -> p c", p=128), writes=[gom_cb])

            def branch_norm(yT, yTb, gc, gcb):
                for st in range(4):
                    cols = slice(st * 512, (st + 1) * 512)
                    S.op("act", lambda e: e.activation(out=sqn[:, 0:4, :], in_=yT[:, :, cols], func=AF.Square), reads=[yTb], writes=[sqnb])
                    pn, pnb = PS[7]
                    for c in range(4):
                        mm(pn[:], ones_bf[:], sqn[:, c, :], c == 0, c == 3, [ones_bfb, sqnb], pnb)
                    rsqrt_to(rtmp[:], rtmpb, pn[:], pnb, 1.0 / 512, rtmp[:], rtmpb)
                    for c in range(4):
                        S.op("dve", lambda e: e.scalar_tensor_tensor(out=yT[:, c, cols], in0=yT[:, c, cols], scalar=gc[:, c:c + 1], in1=rtmp[:],
                                                                     op0=ALU.mult, op1=ALU.mult), reads=[yTb, gcb, rtmpb], writes=[yTb])

            branch_norm(ymT, ymTb, gom_c, gom_cb)
            stop_here("M")
            S.barrier()
            pm.close()
            pam.close()
            ps_ = newstack()
            WS, WSb = sbp(ps_, "WS", [128, 32, 128], BF16)
            WSs, WSsb = sbp(ps_, "WSs", [128, 32, 128], BF16)
            WY, WYb = sbp(ps_, "WY", [128, 32, 128], BF16)
            WT, WTb = sbp(ps_, "WT", [128, 32, 128], BF16)
            Est, Estb = sbp(ps_, "Est", [128, 8, 8, 128], BF16)
            th8, th8b = sbp(ps_, "th8", [128, 32], F32)
            r8, r8b = sbp(ps_, "r8", [128, 32], F32)
            S.op("pool", lambda e: e.memset(Est[:], 1.0), writes=[Estb])
            S.op("pool", lambda e: e.affine_select(out=Est[:], in_=Est[:], pattern=[[-16, 8], [0, 8], [0, 8], [-1, 16]], compare_op=ALU.is_equal,
                                                   fill=0.0, base=0, channel_multiplier=1), reads=[Estb], writes=[Estb])
            S.op("pool", lambda e: e.affine_select(out=Est[:], in_=Est[:], pattern=[[0, 8], [1, 8], [-1, 8], [0, 16]], compare_op=ALU.is_equal,
                                                   fill=0.0, base=0, channel_multiplier=0), reads=[Estb], writes=[Estb])
            stop_here("S0")
            pp = newstack()
            lre, lreb = sbp(pp, "lre", [128, 32], F32)
            lim, limb = sbp(pp, "lim", [128, 32], F32)
            dtt, dttb = sbp(pp, "dtt", [128, 32], F32)
            Bre, Breb = sbp(pp, "Bre", [128, 32, 16], F32)
            Bim, Bimb = sbp(pp, "Bim", [128, 32, 16], F32)
            Cre, Creb = sbp(pp, "Cre", [128, 32, 16], F32)
            Cim, Cimb = sbp(pp, "Cim", [128, 32, 16], F32)
            cld, cldb = sbp(pp, "cld", [128, 4, 64], F32)
            dcol, dcolb = sbp(pp, "dcol", [128, 32], F32)
            with nc.allow_non_contiguous_dma("small ssm param loads"):
                for hf in range(2):
                    S.dma("sp", lre[hf * 64:(hf + 1) * 64, :], lam_re.rearrange("g p -> p g"), writes=[lreb])
                    S.dma("sp", lim[hf * 64:(hf + 1) * 64, :], lam_im.rearrange("g p -> p g"), writes=[limb])
                    S.dma("sp", Bre[hf * 64:(hf + 1) * 64, :, :], b_re.rearrange("g p h -> p g h"), writes=[Breb])
                    S.dma("sp", Bim[hf * 64:(hf + 1) * 64, :, :], b_im.rearrange("g p h -> p g h"), writes=[Bimb])
                for j in range(8):
                    S.dma("sp", dcol[j * 16:(j + 1) * 16, :], ssm_d.rearrange("(g h) -> h g", h=16), writes=[dcolb])
            S.dma("sp", dtt[:], log_dt.partition_broadcast(128), writes=[dttb])
            for (src, dst, dstb) in ((c_re, Cre, Creb), (c_im, Cim, Cimb)):
                S.dma("sp", cld[:], src.rearrange("g h p -> (g h) p").rearrange("(t q) p -> q t p", q=128), writes=[cldb])
                pc, pcb = PS[0]
                for t in range(4):
                    S.op("pe", lambda e: e.transpose(pc[0:64, t * 128:(t + 1) * 128], cld[:, t, :], ident_f[:]), reads=[cldb, ident_fb], writes=[pcb])
                S.op("dve", lambda e: e.tensor_copy(out=dst[0:64, :, :].rearrange("p g h -> p (g h)"), in_=pc[0:64, :]), reads=[pcb], writes=[dstb])
                S.op("dve", lambda e: e.tensor_copy(out=dst[64:128, :, :].rearrange("p g h -> p (g h)"), in_=pc[0:64, :]), reads=[pcb], writes=[dstb])
            S.op("act", lambda e: e.activation(out=dtt[:], in_=dtt[:], func=AF.Exp), reads=[dttb], writes=[dttb])
            S.op("dve", lambda e: e.tensor_scalar(out=lre[:], in0=lre[:], scalar1=-1e-4, scalar2=None, op0=ALU.min), reads=[lreb], writes=[lreb])
            stop_here("S1")
            x1, x1b = sbp(pp, "x1", [128, 32], F32)
            a1, a1b = sbp(pp, "a1", [128, 32], F32)
            S.op("dve", lambda e: e.tensor_tensor(out=x1[:], in0=lre[:], in1=dtt[:], op=ALU.mult), reads=[lreb, dttb], writes=[x1b])
            S.op("dve", lambda e: e.tensor_tensor(out=a1[:], in0=lim[:], in1=dtt[:], op=ALU.mult), reads=[limb, dttb], writes=[a1b])
            S.op("dve", lambda e: e.tensor_scalar(out=a1[:], in0=a1[:], scalar1=1.0 / TWO_PI, scalar2=None, op0=ALU.mult), reads=[a1b], writes=[a1b])
            kvi, kvib = sbp(pp, "kvi", [128, 16], I32)
            kv, kvb_ = sbp(pp, "kv", [128, 16], F32)
            S.op("pool", lambda e: e.iota(kvi[:, 0:8], pattern=[[1, 8]], base=1, channel_multiplier=0), writes=[kvib])
            S.op("pool", lambda e: e.iota(kvi[:, 8:16], pattern=[[-1, 8]], base=-1, channel_multiplier=0), reads=[kvib], writes=[kvib])
            S.op("dve", lambda e: e.tensor_copy(out=kv[:], in_=kvi[:]), reads=[kvib], writes=[kvb_])
            KX, KXb = sbp(pp, "KX", [128, 16, 32], F32)
            MAG, MAGb = sbp(pp, "MAGt", [128, 16, 32], F32)
            YY, YYb = sbp(pp, "YY", [128, 16, 32], F32)
            Y2, Y2b = sbp(pp, "Y2", [128, 16, 32], F32)
            T1, T1b = sbp(pp, "T1", [128, 16, 32], F32)
            T2, T2b = sbp(pp, "T2", [128, 16, 32], F32)
            PWr, PWrb = sbp(pp, "PWr", [128, 16, 32], F32)
            PWi, PWib = sbp(pp, "PWi", [128, 16, 32], F32)
            kvB = kv[:, :].unsqueeze(2).broadcast_to([128, 16, 32])
            S.op("dve", lambda e: e.tensor_tensor(out=KX[:], in0=kvB, in1=x1[:, :].unsqueeze(1).broadcast_to([128, 16, 32]), op=ALU.mult),
                 reads=[kvb_, x1b], writes=[KXb])
            S.op("act", lambda e: e.activation(out=MAG[:], in_=KX[:], func=AF.Exp), reads=[KXb], writes=[MAGb])
            S.op("dve", lambda e: e.tensor_tensor(out=YY[:], in0=kvB, in1=a1[:, :].unsqueeze(1).broadcast_to([128, 16, 32]), op=ALU.mult),
                 reads=[kvb_, a1b], writes=[YYb])
            fl = lambda t: t[:, :, :].rearrange("p a b -> p (a b)")
            range_reduce_sin(fl(PWi), PWib, fl(YY), YYb, fl(T1), T1b, fl(T2), T2b)
            range_reduce_sin(fl(PWr), PWrb, fl(YY), YYb, fl(T1), T1b, fl(T2), T2b, pre=0.25)
            S.op("dve", lambda e: e.tensor_tensor(out=PWr[:], in0=PWr[:], in1=MAG[:], op=ALU.mult), reads=[PWrb, MAGb], writes=[PWrb])
            S.op("dve", lambda e: e.tensor_tensor(out=PWi[:], in0=PWi[:], in1=MAG[:], op=ALU.mult), reads=[PWib, MAGb], writes=[PWib])
            S.op("dve", lambda e: e.tensor_copy(out=r8[:], in_=MAG[:, 7, :]), reads=[MAGb], writes=[r8b])
            S.op("dve", lambda e: e.tensor_scalar(out=th8[0:64, :], in0=a1[0:64, :], scalar1=8.0, scalar2=None, op0=ALU.mult), reads=[a1b], writes=[th8b])
            S.op("dve", lambda e: e.tensor_scalar(out=th8[64:128, :], in0=a1[64:128, :], scalar1=-8.0, scalar2=None, op0=ALU.mult), reads=[a1b], writes=[th8b])
            stop_here("S2")
            sc = [sbp(pp, "sc%d" % i, [128, 32], F32) for i in range(6)]

            def tt(o, a, b, op, eng="dve"):
                S.op(eng, lambda e: e.tensor_tensor(out=o[0], in0=a[0], in1=b[0], op=op), reads=[a[1], b[1]], writes=[o[1]])

            def V(t, b, ap=None):
                return (t[:] if ap is None else ap, b)

            are = (PWr[:, 0, :], PWrb)
            aim = (PWi[:, 0, :], PWib)
            nr_, den, rden, cre, cim, tmpc = [(sc[i][0][:], sc[i][1]) for i in range(6)]
            S.op("dve", lambda e: e.tensor_scalar(out=nr_[0], in0=are[0], scalar1=-1.0, scalar2=None, op0=ALU.add), reads=[PWrb], writes=[nr_[1]])
            tt(den, (lre[:], lreb), (lre[:], lreb), ALU.mult)
            tt(tmpc, (lim[:], limb), (lim[:], limb), ALU.mult)
            tt(den, den, tmpc, ALU.add)
            S.op("dve", lambda e: e.reciprocal(out=rden[0], in_=den[0]), reads=[den[1]], writes=[rden[1]])
            tt(cre, nr_, (lre[:], lreb), ALU.mult)
            tt(tmpc, aim, (lim[:], limb), ALU.mult)
            tt(cre, cre, tmpc, ALU.add)
            tt(cre, cre, rden, ALU.mult)
            tt(cim, aim, (lre[:], lreb), ALU.mult)
            tt(tmpc, nr_, (lim[:], limb), ALU.mult)
            tt(cim, cim, tmpc, ALU.subtract)
            tt(cim, cim, rden, ALU.mult)
            BBr, BBrb = sbp(pp, "BBr", [128, 32, 16], F32)
            BBi, BBib = sbp(pp, "BBi", [128, 32, 16], F32)
            tb1, tb1b = sbp(pp, "tb1", [128, 32, 16], F32)
            creB = (cre[0].unsqueeze(2).broadcast_to([128, 32, 16]), cre[1])
            cimB = (cim[0].unsqueeze(2).broadcast_to([128, 32, 16]), cim[1])
            tt((BBr[:], BBrb), (Bre[:], Breb), creB, ALU.mult)
            tt((tb1[:], tb1b), (Bim[:], Bimb), cimB, ALU.mult)
            tt((BBr[:], BBrb), (BBr[:], BBrb), (tb1[:], tb1b), ALU.subtract)
            tt((BBi[:], BBib), (Bre[:], Breb), cimB, ALU.mult)
            tt((tb1[:], tb1b), (Bim[:], Bimb), creB, ALU.mult)
            tt((BBi[:], BBib), (BBi[:], BBib), (tb1[:], tb1b), ALU.add)
            stop_here("S3")
            NGH = 8
            big = [sbp(pp, "big%d" % i, [128, NGH, 8, 16], F32) for i in range(8)]
            G1, G2, G3, G4, G5, G6, G7, G8 = [(b[0][:], b[1]) for b in big]
            L2b, L2bb = sbp(pp, "L2b", [128, NGH, 128], BF16)
            Vb, Vbb = sbp(pp, "Vb", [128, NGH, 128], BF16)
            Vs, Vsb = sbp(pp, "Vs", [128, NGH, 128], BF16)
            wtf, wtfb = sbp(pp, "wtf", [128, 128], F32)

            def cmul(outr, outi, ar, ai, br, bi, t1, t2):
                t3_, t4_ = G7, G8
                tt(t1, ar, br, ALU.mult)
                tt(t2, ai, bi, ALU.mult)
                tt(outr, t1, t2, ALU.subtract)
                tt(t3_, ar, bi, ALU.mult, eng="pool")
                tt(t4_, ai, br, ALU.mult, eng="pool")
                tt(outi, t3_, t4_, ALU.add, eng="pool")

            def flat(ap):
                return ap.rearrange("p g j h -> p g (j h)")

            for gh in range(32 // NGH):
                g0 = gh * NGH
                gs = slice(g0, g0 + NGH)

                def pw(t, tb, lo):
                    return (t[:, lo:lo + 8, gs].rearrange("p k g -> p g k").unsqueeze(3).broadcast_to([128, NGH, 8, 16]), tb)

                def bj(t, tb):
                    return (t[:, gs, :].unsqueeze(2).broadcast_to([128, NGH, 8, 16]), tb)

                cmul(G1, G2, bj(Cre, Creb), bj(Cim, Cimb), pw(PWr, PWrb, 0), pw(PWi, PWib, 0), G3, G4)
                S.op("act", lambda e: e.copy(out=WY[0:64, gs, :], in_=flat(G1[0])[0:64]), reads=[G1[1]], writes=[WYb])
                S.op("act", lambda e: e.mul(out=WY[64:128, gs, :], in_=flat(G2[0])[64:128], mul=-1.0), reads=[G2[1]], writes=[WYb])
                cmul(G1, G2, pw(PWr, PWrb, 8), pw(PWi, PWib, 8), bj(BBr, BBrb), bj(BBi, BBib), G3, G4)
                S.op("act", lambda e: e.copy(out=L2b[0:64, :, :], in_=flat(G1[0])[0:64]), reads=[G1[1]], writes=[L2bb])
                S.op("act", lambda e: e.copy(out=L2b[64:128, :, :], in_=flat(G2[0])[64:128]), reads=[G2[1]], writes=[L2bb])
                a8r = (PWr[:, 7, gs].unsqueeze(2).unsqueeze(3).broadcast_to([128, NGH, 8, 16]), PWrb)
                a8i = (PWi[:, 7, gs].unsqueeze(2).unsqueeze(3).broadcast_to([128, NGH, 8, 16]), PWib)
                cmul(G5, G6, a8r, a8i, G1, G2, G3, G4)
                S.op("act", lambda e: e.copy(out=Vb[0:64, :, :], in_=flat(G5[0])[0:64]), reads=[G5[1]], writes=[Vbb])
                S.op("act", lambda e: e.copy(out=Vb[64:128, :, :], in_=flat(G6[0])[64:128]), reads=[G6[1]], writes=[Vbb])
                S.op("act", lambda e: e.copy(out=Vs[0:64, :, :], in_=flat(G6[0])[0:64]), reads=[G6[1]], writes=[Vsb])
                S.op("act", lambda e: e.copy(out=Vs[64:128, :, :], in_=flat(G5[0])[64:128]), reads=[G5[1]], writes=[Vsb])
                for gg in range(NGH):
                    g = g0 + gg
                    pa_, pab_ = PS[g % 2]
                    pab16 = pa_[:].bitcast(BF16)
                    S.op("pe", lambda e: e.transpose(pab16[:, 0:128], Vb[:, gg, :], ident[:]), reads=[Vbb, identb], writes=[pab_])
                    S.op("pe", lambda e: e.transpose(pab16[:, 128:256], Vs[:, gg, :], ident[:]), reads=[Vsb, identb], writes=[pab_])
                    S.op("act", lambda e: e.copy(out=WS[:, g, :], in_=pab16[:, 0:128]), reads=[pab_], writes=[WSb])
                    S.op("act", lambda e: e.copy(out=WSs[:, g, :], in_=pab16[:, 128:256]), reads=[pab_], writes=[WSsb])
                    pw_t, pwb_ = PS[2 + (g % 2)]
                    mm(pw_t[:, 0:128], L2b[:, gg, :], WY[:, g, :], True, True, [L2bb, WYb], pwb_)
                    S.op("dve", lambda e: e.scalar_tensor_tensor(out=wtf[:], in0=ident_f[:], scalar=dcol[:, g:g + 1], in1=pw_t[:, 0:128],
                                                                 op0=ALU.mult, op1=ALU.add), reads=[ident_fb, dcolb, pwb_], writes=[wtfb])
                    S.op("pool", lambda e: e.affine_select(out=WT[:, g, :].rearrange("p (j h) -> p j h", j=8), in_=wtf[:].rearrange("p (j h) -> p j h", j=8),
                                                           pattern=[[16, 8], [0, 16]], compare_op=ALU.is_ge, fill=0.0, base=15, channel_multiplier=-1),
                         reads=[wtfb], writes=[WTb])
            stop_here("S4")
            S.barrier()
            pp.close()
            Eun, Eunb = sbp(ps_, "Eun", [128, 8, 8, 128], BF16)
            S.op("pool", lambda e: e.memset(Eun[:], 1.0), writes=[Eunb])
            S.op("pool", lambda e: e.affine_select(out=Eun[:], in_=Eun[:], pattern=[[0, 8], [-16, 8], [0, 8], [-1, 16]], compare_op=ALU.is_equal,
                                                   fill=0.0, base=0, channel_multiplier=1), reads=[Eunb], writes=[Eunb])
            S.op("pool", lambda e: e.affine_select(out=Eun[:], in_=Eun[:], pattern=[[1, 8], [0, 8], [-1, 8], [0, 16]], compare_op=ALU.is_equal,
                                                   fill=0.0, base=0, channel_multiplier=0), reads=[Eunb], writes=[Eunb])
            pl = newstack()
            iot, iotb = sbp(pl, "iot", [128, 512], F32)
            TcB = [sbp(pl, "Tc%d" % i, [128, 2, 512], F32) for i in range(2)]
            TsB = [sbp(pl, "Ts%d" % i, [128, 2, 512], F32) for i in range(2)]
            Yt, Ytb = sbp(pl, "Yt", [128, 2, 512], F32)
            Yu, Yub = sbp(pl, "Yu", [128, 2, 512], F32)
            Yv, Yvb = sbp(pl, "Yv", [128, 2, 512], F32)
            Yw, Ywb = sbp(pl, "Yw", [128, 2, 512], F32)
            ioti = Yw[:, 0, :].bitcast(I32)
            S.op("pool", lambda e: e.iota(ioti, pattern=[[1, 512]], base=1, channel_multiplier=0), writes=[Ywb])
            S.op("dve", lambda e: e.tensor_copy(out=iot[:], in_=ioti), reads=[Ywb], writes=[iotb])
            Ust = [sbp(pl, "Ust%d" % i, [128, 512], BF16) for i in range(3)]
            sA = [sbp(pl, "sA%d" % i, [128, 512], F32) for i in range(2)]
            sB = [sbp(pl, "sB%d" % i, [128, 512], F32) for i in range(2)]
            St_ = [sbp(pl, "St%d" % i, [128, 512], F32) for i in range(2)]
            xs = [sbp(pl, "xs%d" % i, [128, 512], F32) for i in range(2)]
            xsb16 = [sbp(pl, "xsb%d" % i, [128, 256], BF16) for i in range(2)]
            t3 = [sbp(pl, "t3%d" % i, [128, 256], F32) for i in range(2)]
            t4 = [sbp(pl, "t4%d" % i, [128, 256], F32) for i in range(2)]
            Xb = [sbp(pl, "Xb%d" % i, [128, 256], BF16) for i in range(2)]
            Yst, Ystb = sbp(pl, "Yst", [128, 8, 256], BF16)
            ypre, ypreb = ysT, ysTb
            f4 = lambda t: t[:, :, :].rearrange("p a b -> p (a b)")
            OWN = slice(255, 511)
            def stage0(g):
                c, gl = g // 8, g % 8
                U_t, U_b = Ust[g % 3]
                pu, pub = PS[g % 2]
                uview = uT[:, c, :, :]
                for j in range(8):
                    mm(pu[:], Est[:, gl, j, :], uview[:, j, :], j == 0, j == 7, [Estb, uTb], pub)
                S.op("act", lambda e: e.copy(out=U_t[:], in_=pu[:]), reads=[pub], writes=[U_b])

            def stage1a(g):
                U_t, U_b = Ust[g % 3]
                p1, p1b = PS[2 + (g % 2)]
                p2, p2b = PS[4 + (g % 2)]
                mm(p1[:], WS[:, g, :], U_t[:], True, True, [WSb, U_b], p1b)
                mm(p2[:], WSs[:, g, :], U_t[:], True, True, [WSsb, U_b], p2b)

            def stage1b(g):
                gi = g % 2
                Tc, Tcb = TcB[(g // 2) % 2]
                Ts, Tsb = TsB[(g // 2) % 2]
                if gi == 0:
                    S.op("dve", lambda e: e.tensor_tensor(out=Yt[:], in0=iot[:, :].unsqueeze(1).broadcast_to([128, 2, 512]),
                                                          in1=th8[:, g:g + 2].unsqueeze(2).broadcast_to([128, 2, 512]), op=ALU.mult),
                         reads=[iotb, th8b], writes=[Ytb])
                    range_reduce_sin(f4(Ts), Tsb, f4(Yt), Ytb, f4(Yv), Yvb, f4(Yw), Ywb)
                    range_reduce_sin(f4(Tc), Tcb, f4(Yt), Ytb, f4(Yu), Yub, f4(Yv), Yvb, pre=0.25)
                p1, p1b = PS[2 + (g % 2)]
                p2, p2b = PS[4 + (g % 2)]
                a_t, a_b = sA[g % 2]
                b_t, b_b = sB[g % 2]
                s_t, s_b = St_[g % 2]
                x_t, x_b = xs[g % 2]
                S.op("dve", lambda e: e.tensor_tensor(out=a_t[:], in0=p1[:], in1=Tc[:, gi, :], op=ALU.mult), reads=[p1b, Tcb], writes=[a_b])
                S.op("dve", lambda e: e.tensor_tensor(out=b_t[:], in0=p2[:], in1=Ts[:, gi, :], op=ALU.mult), reads=[p2b, Tsb], writes=[b_b])
                S.op("dve", lambda e: e.tensor_tensor(out=s_t[:], in0=a_t[:], in1=b_t[:], op=ALU.add), reads=[a_b, b_b], writes=[s_b])
                S.op("dve", lambda e: e.tensor_tensor_scan(out=x_t[:], data0=r8[:, g:g + 1].broadcast_to([128, 512]), data1=s_t[:], initial=0.0,
                                                           op0=ALU.mult, op1=ALU.add), reads=[r8b, s_b], writes=[x_b])
                xb_t, xb_b = xsb16[g % 2]
                S.op("act", lambda e: e.copy(out=xb_t[:], in_=x_t[:, OWN]), reads=[x_b], writes=[xb_b])

            def stage2(g):
                c, gl = g // 8, g % 8
                gi = g % 2
                Tc, Tcb = TcB[(g // 2) % 2]
                Ts, Tsb = TsB[(g // 2) % 2]
                U_t, U_b = Ust[g % 3]
                x_t, x_b = xs[g % 2]
                xb_t, xb_b = xsb16[g % 2]
                p1, p1b = PS[2 + (g % 2)]
                p3, p3b = PS[6 + (g % 2)]
                mm(p3[:, 0:256], swapm[:], xb_t[:], True, True, [swapmb, xb_b], p3b)
                t3t, t3b = t3[g % 2]
                t4t, t4b = t4[g % 2]
                X_t, X_b = Xb[g % 2]
                S.op("dve", lambda e: e.tensor_tensor(out=t3t[:], in0=x_t[:, OWN], in1=Tc[:, gi, OWN], op=ALU.mult), reads=[x_b, Tcb], writes=[t3b])
                S.op("dve", lambda e: e.tensor_tensor(out=t4t[:], in0=p3[:, 0:256], in1=Ts[:, gi, OWN], op=ALU.mult), reads=[p3b, Tsb], writes=[t4b])
                S.op("dve", lambda e: e.tensor_tensor(out=X_t[:], in0=t3t[:], in1=t4t[:], op=ALU.subtract), reads=[t3b, t4b], writes=[X_b])
                mm(p1[:, 0:256], WY[:, g, :], X_t[:], True, False, [WYb, X_b], p1b)
                mm(p1[:, 0:256], WT[:, g, :], U_t[:, 256:512], False, True, [WTb, U_b], p1b)
                S.op("act", lambda e: e.copy(out=Yst[:, gl, :], in_=p1[:, 0:256]), reads=[p1b], writes=[Ystb])
                if gl == 7:
                    yview = ypre[:, c, :].rearrange("p (m j) -> p j m", j=8)
                    for j in range(8):
                        pj, pjb = PS[j % 2]
                        for g2 in range(8):
                            mm(pj[:, 0:256], Eun[:, g2, j, :], Yst[:, g2, :], g2 == 0, g2 == 7, [Eunb, Ystb], pjb)
                        S.op("act", lambda e: e.copy(out=yview[:, j, :], in_=pj[:, 0:256]), reads=[pjb], writes=[ypreb])

            stage0(0)
            stage0(1)
            stage1a(0)
            stage1b(0)
            for g in range(32):
                if g + 1 < 32:
                    stage1a(g + 1)
                if g + 2 < 32:
                    stage0(g + 2)
                if g + 1 < 32:
                    stage1b(g + 1)
                stage2(g)
            stop_here("S5", ysT[:, :, 0:512], ysTb)
            S.barrier()
            pl.close()
            pl = newstack()
            wglu, wglub = sbp(pl, "wglu", [128, 4, 512], BF16)
            for c in range(4):
                S.dma("pool", wglu[:, c, :], w_glu[c * 128:(c + 1) * 128, :], writes=[wglub])
            bglu_c, bglu_cb = sbp(pl, "bglu_c", [128, 4], F32)
            gos_c, gos_cb = sbp(pl, "gos_c", [128, 4], F32)
            with nc.allow_non_contiguous_dma("small param loads"):
                S.dma("sp", bglu_c[:], b_glu.rearrange("(c p) -> p c", p=128), writes=[bglu_cb])
                S.dma("sp", gos_c[:], g_os.rearrange("(c p) -> p c", p=128), writes=[gos_cb])
            g1, g1b = sbp(pl, "g1", [128, 4, 512], F32)
            g2_, g2b = sbp(pl, "g2", [128, 4, 512], F32)
            y1, y1b = sbp(pl, "y1", [128, 4, 512], BF16)
            sg, sgb = sbp(pl, "sg", [128, 512], F32)
            for st in range(4):
                cols = slice(st * 512, (st + 1) * 512)
                yv = ypre[:, :, cols]
                S.op("dve", lambda e: e.tensor_tensor(out=g1[:], in0=yv, in1=yv, op=ALU.mult), reads=[ypreb], writes=[g1b])
                S.op("dve", lambda e: e.tensor_scalar(out=g1[:], in0=g1[:], scalar1=0.044715, scalar2=1.0, op0=ALU.mult, op1=ALU.add), reads=[g1b], writes=[g1b])
                S.op("dve", lambda e: e.tensor_tensor(out=g2_[:], in0=g1[:], in1=yv, op=ALU.mult), reads=[g1b, ypreb], writes=[g2b])
                S.op("act", lambda e: e.activation(out=g1[:], in_=g2_[:], func=AF.Sigmoid, scale=1.5957691216057308), reads=[g2b], writes=[g1b])
                S.op("dve", lambda e: e.tensor_tensor(out=y1[:], in0=g1[:], in1=yv, op=ALU.mult), reads=[g1b, ypreb], writes=[y1b])
                for c2 in range(4):
                    pg, pgb = PS[c2 % 2]
                    for c in range(4):
                        mm(pg[:], wglu[:, c, c2 * 128:(c2 + 1) * 128], y1[:, c, :], c == 0, c == 3, [wglub, y1b], pgb)
                    S.op("act", lambda e: e.activation(out=sg[:], in_=pg[:], func=AF.Sigmoid, bias=bglu_c[:, c2:c2 + 1]), reads=[pgb, bglu_cb], writes=[sgb])
                    S.op("dve", lambda e: e.tensor_tensor(out=ysT[:, c2, cols], in0=y1[:, c2, :], in1=sg[:], op=ALU.mult), reads=[y1b, sgb], writes=[ysTb])
            stop_here("S6", ysT[:, :, 0:512], ysTb)
            branch_norm(ysT, ysTb, gos_c, gos_cb)
            stop_here("S7", ysT[:, :, 0:512], ysTb)
            S.barrier()
            pl.close()
            ps_.close()
            stackU.close()
            rest = newstack()
            xres, xresb = sbp(rest, "xres", [128, 16, D], F32)
            po_ = newstack()
            wout, woutb = sbp(po_, "wout", [128, 8, D], BF16)
            for c in range(8):
                S.dma("pool", wout[:, c, :], w_out[c * 128:(c + 1) * 128, :], writes=[woutb])
            xrb = [Buf("xres%d" % i) for i in range(16)]
            for i in range(16):
                S.dma("sp", xres[:, i, :], xa[T_OWN + i * 128:T_OWN + (i + 1) * 128, :], writes=[xrb[i]])
                for hf in range(2):
                    pt, pb = PS[(2 * i + hf) % 4]
                    for c in range(8):
                        src, srcb = (ysT, ysTb) if c < 4 else (ymT, ymTb)
                        mm(pt[:], src[:, c % 4, i * 128:(i + 1) * 128], wout[:, c, hf * 512:(hf + 1) * 512], c == 0, c == 7, [srcb, woutb], pb)
                    S.op("dve", lambda e: e.tensor_tensor(out=xres[:, i, hf * 512:(hf + 1) * 512], in0=pt[:], in1=xres[:, i, hf * 512:(hf + 1) * 512], op=ALU.add),
                         reads=[pb, xrb[i]], writes=[xrb[i]])
            stop_here("O", xres[:, 0:4, 0:512], xrb[0])
            S.barrier()
            po_.close()

            ph2 = newstack()
            gb2, gb2b = sbp(ph2, "gb2", [128, D], F32)
            hb2 = [sbp(ph2, "hb2_%d" % i, [128, D], BF16) for i in range(2)]
            sq2, sq2b = sbp(ph2, "sq2", [128, D], BF16)
            st2 = [sbp(ph2, "st2_%d" % i, [128, 4], F32) for i in range(4)]
            cnt2 = [0]

            def norm_T(x_ap, x_b, g_t, g_b, dstT, dstTb, col0, nfeat=D, want_h=False, h_dst=None):
                i = cnt2[0]
                cnt2[0] += 1
                h_t, h_b = hb2[i % 2]
                h_ap = h_t[:]
                if h_dst is not None:
                    h_ap, h_b = h_dst
                s_t, s_b = st2[i % 4]
                S.op("act", lambda e: e.activation(out=sq2[:], in_=x_ap, func=AF.Square, accum_out=s_t[:, 0:1]), reads=[x_b], writes=[sq2b, s_b])
                S.op("act", lambda e: e.activation(out=s_t[:, 1:2], in_=s_t[:, 0:1], func=AF.Sqrt, bias=eps_c[:], scale=1.0 / nfeat),
                     reads=[s_b, eps_cb], writes=[s_b])
                S.op("dve", lambda e: e.reciprocal(out=s_t[:, 2:3], in_=s_t[:, 1:2]), reads=[s_b], writes=[s_b])
                S.op("dve", lambda e: e.scalar_tensor_tensor(out=h_ap, in0=x_ap, scalar=s_t[:, 2:3], in1=g_t[:], op0=ALU.mult, op1=ALU.mult),
                     reads=[x_b, s_b, g_b], writes=[h_b])
                if dstT is None:
                    return h_t, h_b, s_t, s_b
                pt, pb = PS[i % 2]
                ptb = pt[:].bitcast(BF16)
                for c in range(8):
                    S.op("pe", lambda e: e.transpose(ptb[:, c * 128:(c + 1) * 128], h_ap[:, c * 128:(c + 1) * 128], ident[:]), reads=[h_b, identb], writes=[pb])
                S.op("act", lambda e: e.copy(out=dstT[:, :, col0:col0 + 128], in_=ptb.rearrange("p (c t) -> p c t", c=8)), reads=[pb], writes=[dstTb])
                if want_h:
                    return h_t, h_b, s_t, s_b

            def load_w(ph, name, src, kch, n):
                t, b = sbp(ph, name, [128, kch, n], BF16)
                for c in range(kch):
                    S.dma("pool", t[:, c, :], src[c * 128:(c + 1) * 128, :], writes=[b])
                return t, b

            px = newstack()
            S.dma("sp", gb2[:], g_mem.partition_broadcast(128), writes=[gb2b])
            memT, memTb = sbp(px, "memT", [128, 8, 256], BF16)
            mt, mtb = sbp(px, "mt", [128, D], F32)
            for i in range(2):
                S.dma("sp", mt[:], mem[i * 128:(i + 1) * 128, :], writes=[mtb])
                norm_T(mt[:], mtb, gb2, gb2b, memT, memTb, i * 128)
            wk_, wkb_ = load_w(px, "xwk", xw_k, 8, D)
            KmT, KmTb = sbp(px, "KmT", [128, 8, 256], BF16)
            for cc in range(8):
                pt, pb = PS[2 + cc % 2]
                for k in range(8):
                    mm(pt[:, 0:256], wk_[:, k, cc * 128:(cc + 1) * 128], memT[:, k, :], k == 0, k == 7, [wkb_, memTb], pb)
                S.op("act", lambda e: e.copy(out=KmT[:, cc, :], in_=pt[:, 0:256]), reads=[pb], writes=[KmTb])
            wv_, wvb_ = wk_, wkb_
            for c in range(8):
                S.dma("pool", wv_[:, c, :], xw_v[c * 128:(c + 1) * 128, :], writes=[wvb_])
            Vm, Vmb = sbp(px, "Vm", [128, 2, D], BF16)
            for mtile in range(2):
                for hf in range(2):
                    pt, pb = PS[2 + hf]
                    for k in range(8):
                        mm(pt[:], memT[:, k, mtile * 128:(mtile + 1) * 128], wv_[:, k, hf * 512:(hf + 1) * 512], k == 0, k == 7, [wvb_, memTb], pb)
                    S.op("act", lambda e: e.copy(out=Vm[:, mtile, hf * 512:(hf + 1) * 512], in_=pt[:]), reads=[pb], writes=[Vmb])
            wq_, wqb_ = wk_, wkb_
            for c in range(8):
                S.dma("pool", wq_[:, c, :], xw_q[c * 128:(c + 1) * 128, :], writes=[wqb_])
            wo_, wob_ = load_w(px, "xwo", xw_o, 8, D)
            S.dma("sp", gb2[:], g_xa.partition_broadcast(128), reads=[], writes=[gb2b])
            h2TL = [sbp(px, "h2T%d" % i, [128, 8, 512], BF16) for i in range(2)]
            QxT, QxTb = sbp(px, "QxT", [128, 8, 512], BF16)
            oT, oTb = sbp(px, "oT", [128, 8, 512], BF16)
            PTx = [sbp(px, "PTx%d" % i, [128, 512], BF16) for i in range(4)]
            rlx, rlxb = sbp(px, "rlx", [128, 512], F32)
            for ti in range(4):
                norm_T(xres[:, ti, :], xrb[ti], gb2, gb2b, h2TL[0][0], h2TL[0][1], ti * 128)
            for st in range(4):
                h2T, h2Tb = h2TL[st % 2]
                for cc in range(8):
                    pt, pb = PS[2 + cc % 2]
                    for k in range(8):
                        mm(pt[:], wq_[:, k, cc * 128:(cc + 1) * 128], h2T[:, k, :], k == 0, k == 7, [wqb_, h2Tb], pb)
                    S.op("act", lambda e: e.copy(out=QxT[:, cc, :], in_=pt[:]), reads=[pb], writes=[QxTb])
                def x_scores(hh):
                    banks = (PS[4], PS[5]) if hh % 2 == 0 else (PS[6], PS[7])
                    for mb in range(2):
                        pt, pb = banks[mb]
                        P_t, P_b = PTx[(hh % 2) * 2 + mb]
                        for dc in range(2):
                            mm(pt[:], KmT[:, 2 * hh + dc, mb * 128:(mb + 1) * 128], QxT[:, 2 * hh + dc, :], dc == 0, dc == 1, [KmTb, QxTb], pb)
                        S.op("act", lambda e: e.activation(out=P_t[:], in_=pt[:], func=AF.Exp, scale=1.0 / 16.0), reads=[pb], writes=[P_b])

                x_scores(0)
                for hh in range(4):
                    if st + 1 < 4:
                        i_n = (st + 1) * 4 + hh
                        norm_T(xres[:, i_n, :], xrb[i_n], gb2, gb2b, h2TL[(st + 1) % 2][0], h2TL[(st + 1) % 2][1], hh * 128)
                    if hh + 1 < 4:
                        x_scores(hh + 1)
                    Pm = [PTx[(hh % 2) * 2 + mb] for mb in range(2)]
                    pl_, plb_ = PS[2]
                    for mb in range(2):
                        mm(pl_[:], ones_bf[:], Pm[mb][0][:], mb == 0, mb == 1, [ones_bfb, Pm[mb][1]], plb_)
                    S.op("dve", lambda e: e.reciprocal(out=rlx[:], in_=pl_[:]), reads=[plb_], writes=[rlxb])
                    for dc in range(2):
                        pt, pb = PS[3]
                        for mb in range(2):
                            mm(pt[:], Vm[:, mb, (2 * hh + dc) * 128:(2 * hh + dc + 1) * 128], Pm[mb][0][:], mb == 0, mb == 1, [Vmb, Pm[mb][1]], pb)
                        S.op("dve", lambda e: e.tensor_tensor(out=oT[:, 2 * hh + dc, :], in0=pt[:], in1=rlx[:], op=ALU.mult), reads=[pb, rlxb], writes=[oTb])
                for ti in range(4):
                    i = st * 4 + ti
                    for hf in range(2):
                        pt, pb = PS[4 + hf]
                        for c in range(8):
                            mm(pt[:], oT[:, c, ti * 128:(ti + 1) * 128], wo_[:, c, hf * 512:(hf + 1) * 512], c == 0, c == 7, [oTb, wob_], pb)
                        S.op("dve", lambda e: e.tensor_tensor(out=xres[:, i, hf * 512:(hf + 1) * 512], in0=pt[:], in1=xres[:, i, hf * 512:(hf + 1) * 512], op=ALU.add),
                             reads=[pb, xrb[i]], writes=[xrb[i]])
            S.barrier()
            px.close()
            stop_here("X", xres[:, 0:4, 0:512], xrb[0])
            TS = MOE_TS
            NT = MOE_NT
            NSUB = TS // 128
            NSLOT = NT * TS
            pe_ = newstack()
            S.dma("sp", gb2[:], g_moe.partition_broadcast(128), writes=[gb2b])
            h3Tt = [sbp(pe_, "h3Tt%d" % i, [128, 8, 128], BF16) for i in range(2)]
            wr, wrb = sbp(pe_, "wr", [128, 8, 36], BF16)
            bb, bbb = sbp(pe_, "bb", [128, 36], F32)
            S.dma("pool", wr[:, :, 0:4], w_group.rearrange("(c p) n -> p c n", p=128), writes=[wrb])
            S.dma("pool", wr[:, :, 4:36], w_expert.rearrange("(c p) n -> p c n", p=128), writes=[wrb])
            S.dma("sp", bb[:, 0:4], b_group.partition_broadcast(128), writes=[bbb])
            S.dma("sp", bb[:, 4:36], b_expert.partition_broadcast(128), writes=[bbb])
            Lst, Lstb = sbp(pe_, "Lst", [128, 128], BF16)
            S.op("pool", lambda e: e.memset(Lst[:], 1.0), writes=[Lstb])
            S.op("pool", lambda e: e.affine_select(out=Lst[:], in_=Lst[:], pattern=[[1, 128]], compare_op=ALU.is_gt, fill=0.0, base=0, channel_multiplier=-1),
                 reads=[Lstb], writes=[Lstb])
            Mall, Mallb = sbp(pe_, "Mall", [128, 16, 32], BF16)
            Moh, Mohb = sbp(pe_, "Moh", [128, 16, 2, 32], F32)
            wts, wtsb = sbp(pe_, "wts", [128, 16, 2], F32)
            idxf, idxfb = sbp(pe_, "idxf", [128, 16, 2], F32)
            idxs, idxsb = sbp(pe_, "idxs", [128, 16, 2], I32)
            rs, rsb = sbp(pe_, "rs", [128, 192], F32)
            lg = rs[:, 0:36]
            gm, ngm, gs_, gw = rs[:, 36:37], rs[:, 37:38], rs[:, 38:39], rs[:, 39:40]
            goh, gex = rs[:, 40:44], rs[:, 44:48]
            t48 = rs[:, 48:80]
            esel, oh1, e2, oh2 = rs[:, 80:88], rs[:, 88:96], rs[:, 96:104], rs[:, 104:112]
            m1, m2, dd, sgm = rs[:, 120:121], rs[:, 121:122], rs[:, 122:123], rs[:, 123:124]
            ptmp = rs[:, 128:160]
            ptmp2 = rs[:, 160:192]
            yscr = nc.dram_tensor("ye_scr", [NSLOT, D], BF16, kind="Internal").ap()

            def R(eng, fn):
                S.op(eng, fn, reads=[rsb], writes=[rsb])

            def h3tile(i):
                return (ymT if i < 8 else ysT)[:, :, :].rearrange("p c t -> p (c t)")[:, (i % 8) * D:(i % 8 + 1) * D]

            h3b = [Buf("h3_%d" % i) for i in range(16)]
            pr1 = newstack()
            LG, LGb = sbp(pr1, "LG", [128, 16, 36], F32)
            for i in range(16):
                hT_t, hT_b = h3Tt[i % 2]
                norm_T(xres[:, i, :], xrb[i], gb2, gb2b, hT_t, hT_b, 0, h_dst=(h3tile(i), h3b[i]))
                pr, prb = PS[2 + i % 2]
                for k in range(8):
                    mm(pr[:, 0:36], hT_t[:, k, :], wr[:, k, :], k == 0, k == 7, [hT_b, wrb], prb)
                S.op("dve", lambda e: e.tensor_tensor(out=LG[:, i, :], in0=pr[:, 0:36], in1=bb[:], op=ALU.add), reads=[prb, bbb], writes=[LGb])
            rb_, rbb = sbp(pr1, "rb_", [128, 16, 64], F32)
            GLv = LG[:, :, 0:4]
            ELv = LG[:, :, 4:36].rearrange("p t (g e) -> p t g e", g=4)
            gmB, gsB, gwB, m1B, m2B, ddB = [rb_[:, :, c] for c in range(6)]
            gohB_, gexB = rb_[:, :, 8:12], rb_[:, :, 12:16]
            eselB, oh1B, e2B, oh2B = rb_[:, :, 16:24], rb_[:, :, 24:32], rb_[:, :, 32:40], rb_[:, :, 40:48]
            t48B, t48Bb = sbp(pr1, "t48B", [128, 16, 4, 8], F32)

            def RB(eng, fn, extra_r=(), extra_w=()):
                S.op(eng, fn, reads=[rbb, LGb] + list(extra_r), writes=[rbb] + list(extra_w))

            def bc4(v):
                return v.unsqueeze(2).broadcast_to([128, 16, 4])

            def bc8(v):
                return v.unsqueeze(2).broadcast_to([128, 16, 8])

            RB("dve", lambda e: e.tensor_reduce(out=gmB, in_=GLv, axis=AX.X, op=ALU.max))
            RB("dve", lambda e: e.tensor_tensor(out=gohB_, in0=GLv, in1=bc4(gmB), op=ALU.is_ge))
            RB("dve", lambda e: e.tensor_tensor(out=gexB, in0=GLv, in1=bc4(gmB), op=ALU.subtract))
            RB("act", lambda e: e.activation(out=gexB, in_=gexB, func=AF.Exp))
            RB("dve", lambda e: e.tensor_reduce(out=gsB, in_=gexB, axis=AX.X, op=ALU.add))
            RB("dve", lambda e: e.reciprocal(out=gwB, in_=gsB))
            S.op("dve", lambda e: e.tensor_tensor(out=t48B[:], in0=ELv, in1=gohB_.unsqueeze(3).broadcast_to([128, 16, 4, 8]), op=ALU.mult),
                 reads=[rbb, LGb], writes=[t48Bb])
            S.op("dve", lambda e: e.tensor_reduce(out=eselB, in_=t48B[:, :, :, :].rearrange("p t g e -> p t e g"), axis=AX.X, op=ALU.add),
                 reads=[t48Bb], writes=[rbb])
            RB("dve", lambda e: e.tensor_reduce(out=m1B, in_=eselB, axis=AX.X, op=ALU.max))
            RB("dve", lambda e: e.tensor_tensor(out=oh1B, in0=eselB, in1=bc8(m1B), op=ALU.is_ge))
            RB("dve", lambda e: e.scalar_tensor_tensor(out=e2B, in0=oh1B, scalar=-1e30, in1=eselB, op0=ALU.mult, op1=ALU.add))
            RB("dve", lambda e: e.tensor_reduce(out=m2B, in_=e2B, axis=AX.X, op=ALU.max))
            RB("dve", lambda e: e.tensor_tensor(out=oh2B, in0=e2B, in1=bc8(m2B), op=ALU.is_ge))
            RB("dve", lambda e: e.tensor_tensor(out=ddB, in0=m2B, in1=m1B, op=ALU.subtract))
            RB("act", lambda e: e.activation(out=ddB, in_=ddB, func=AF.Sigmoid))
            S.op("dve", lambda e: e.tensor_tensor(out=wts[:, :, 1], in0=ddB, in1=gwB, op=ALU.mult), reads=[rbb], writes=[wtsb])
            S.op("dve", lambda e: e.tensor_tensor(out=wts[:, :, 0], in0=gwB, in1=wts[:, :, 1], op=ALU.subtract), reads=[rbb, wtsb], writes=[wtsb])
            for k2, ohB in ((0, oh1B), (1, oh2B)):
                S.op("dve", lambda e: e.tensor_tensor(out=Moh[:, :, k2, :].rearrange("p t (g e) -> p t g e", g=4),
                                                      in0=gohB_.unsqueeze(3).broadcast_to([128, 16, 4, 8]),
                                                      in1=ohB.unsqueeze(2).broadcast_to([128, 16, 4, 8]), op=ALU.mult), reads=[rbb], writes=[Mohb])
            S.op("dve", lambda e: e.tensor_tensor(out=Mall[:, :, :], in0=Moh[:, :, 0, :], in1=Moh[:, :, 1, :], op=ALU.add), reads=[Mohb], writes=[Mallb])
            S.barrier()
            pr1.close()
            pk = [sbp(pe_, "pk%d" % i, [128, 32], F32) for i in range(6)]
            cntf, ntl, incl, soff, ones32, tmpk = [(p[0][:], p[1]) for p in pk]
            pc_, pcb_ = PS[6]
            for i in range(16):
                mm(pc_[:, 0:32], ones_bf[:], Mall[:, i, :], i == 0, i == 15, [ones_bfb, Mallb], pcb_)
            S.op("dve", lambda e: e.tensor_copy(out=cntf[0], in_=pc_[:, 0:32]), reads=[pcb_], writes=[cntf[1]])
            S.op("dve", lambda e: e.tensor_scalar(out=ntl[0], in0=cntf[0], scalar1=0.5, scalar2=None, op0=ALU.is_gt), reads=[cntf[1]], writes=[ntl[1]])
            for th in [TS * q + 0.5 for q in range(1, 2048 // TS)]:
                S.op("dve", lambda e: e.scalar_tensor_tensor(out=ntl[0], in0=cntf[0], scalar=th, in1=ntl[0], op0=ALU.is_gt, op1=ALU.add),
                     reads=[cntf[1], ntl[1]], writes=[ntl[1]])
            S.op("pool", lambda e: e.memset(ones32[0], 1.0), writes=[ones32[1]])
            S.op("dve", lambda e: e.tensor_tensor_scan(out=incl[0], data0=ones32[0], data1=ntl[0], initial=0.0, op0=ALU.mult, op1=ALU.add),
                 reads=[ones32[1], ntl[1]], writes=[incl[1]])
            S.op("dve", lambda e: e.tensor_tensor(out=soff[0], in0=incl[0], in1=ntl[0], op=ALU.subtract), reads=[incl[1], ntl[1]], writes=[soff[1]])
            S.op("dve", lambda e: e.tensor_scalar(out=soff[0], in0=soff[0], scalar1=float(TS), scalar2=None, op0=ALU.mult), reads=[soff[1]], writes=[soff[1]])
            jfi, jfib = sbp(pe_, "jfi", [128, NT], I32)
            jf, jfb = sbp(pe_, "jf", [128, NT], F32)
            pidi, pidib = sbp(pe_, "pidi", [128, 1], I32)
            pidf, pidfb = sbp(pe_, "pidf", [128, 1], F32)
            eidf, eidfb = sbp(pe_, "eidf", [128, NT], F32)
            widx, widxb = sbp(pe_, "widx", [128, NT], I32)
            pcm = newstack()
            cmpT, cmpTb = sbp(pcm, "cmpT", [128, NT, 32], F32)
            S.op("pool", lambda e: e.iota(jfi[:], pattern=[[1, NT]], base=0, channel_multiplier=0), writes=[jfib])
            S.op("dve", lambda e: e.tensor_copy(out=jf[:], in_=jfi[:]), reads=[jfib], writes=[jfb])
            S.op("pool", lambda e: e.iota(pidi[:], pattern=[[0, 1]], base=0, channel_multiplier=1), writes=[pidib])
            S.op("dve", lambda e: e.tensor_copy(out=pidf[:], in_=pidi[:]), reads=[pidib], writes=[pidfb])
            S.op("dve", lambda e: e.tensor_tensor(out=cmpT[:], in0=incl[0].unsqueeze(1).broadcast_to([128, NT, 32]),
                                                  in1=jf[:, :].unsqueeze(2).broadcast_to([128, NT, 32]), op=ALU.is_le), reads=[incl[1], jfb], writes=[cmpTb])
            S.op("dve", lambda e: e.tensor_reduce(out=eidf[:], in_=cmpT[:], axis=AX.X, op=ALU.add), reads=[cmpTb], writes=[eidfb])
            S.op("dve", lambda e: e.tensor_scalar(out=eidf[:], in0=eidf[:], scalar1=32.0, scalar2=128.0, op0=ALU.min, op1=ALU.mult), reads=[eidfb], writes=[eidfb])
            S.op("dve", lambda e: e.tensor_scalar(out=eidf[:], in0=eidf[:], scalar1=pidf[:, 0:1], scalar2=None, op0=ALU.add), reads=[eidfb, pidfb], writes=[eidfb])
            S.op("dve", lambda e: e.tensor_copy(out=widx[:], in_=eidf[:]), reads=[eidfb], writes=[widxb])
            S.barrier()
            pcm.close()
            bc_reg = nc.gpsimd.to_reg(NSLOT - 1)
            bw_reg = nc.gpsimd.to_reg(4095)

            def indirect(out_ap, in_ap, idx_ap, scatter, reads, writes, reg):
                S._deps("pool", reads, writes, is_dma=True)
                k = S.next_dma_sem("pool")
                off = bass.IndirectOffsetOnAxis(ap=idx_ap, axis=0)
                nc.gpsimd.indirect_dma_start(out=out_ap, out_offset=off if scatter else None, in_=in_ap, in_offset=None if scatter else off,
                                             bounds_check=reg, oob_is_err=False).then_inc(S.dsem[k], 16)
                tok = ("d", k, 16 * S.dcnt[k])
                S._mark(tok, reads, writes)
                return tok

            SI = [sbp(pe_, "SI%d" % i, [128, 1], I32) for i in range(4)]
            for tok in zero_toks:
                S._wait("pool", tok)
            si_ = 0
            scat_toks = []
            for i in range(16):
                pp_, ppb_ = PS[4 + i % 2]
                for i2 in range(i):
                    mm(pp_[:, 0:32], ones_bf[:], Mall[:, i2, :], i2 == 0, False, [ones_bfb, Mallb], ppb_)
                mm(pp_[:, 0:32], Lst[:], Mall[:, i, :], i == 0, True, [Lstb, Mallb], ppb_)
                S.op("dve", lambda e: e.tensor_tensor(out=ptmp, in0=pp_[:, 0:32], in1=soff[0], op=ALU.add), reads=[ppb_, soff[1], rsb], writes=[rsb])
                for k2 in range(2):
                    S.op("dve", lambda e: e.tensor_tensor(out=ptmp2, in0=ptmp, in1=Moh[:, i, k2, :], op=ALU.mult), reads=[rsb, Mohb], writes=[rsb])
                    S.op("dve", lambda e: e.tensor_reduce(out=idxf[:, i, k2:k2 + 1], in_=ptmp2, axis=AX.X, op=ALU.add), reads=[rsb], writes=[idxfb])
                S.op("dve", lambda e: e.tensor_copy(out=idxs[:, i, :], in_=idxf[:, i, :]), reads=[idxfb], writes=[idxsb])
                for k2 in range(2):
                    si_t, si_b = SI[si_ % 4]
                    si_ += 1
                    S.op("dve", lambda e: e.tensor_copy(out=si_t[:], in_=idxs[:, i, k2:k2 + 1]), reads=[idxsb], writes=[si_b])
                    scat_toks.append(indirect(xscr[:, :], h3tile(i), si_t[:, :], True, [h3b[i], si_b], [], bc_reg))
            stop_here("R", xres[:, 0:4, 0:512], xrb[0])
            NW = 3
            WG = [sbp(pe_, "WG%d" % i, [128, 8, 256], BF16) for i in range(NW)]
            WU = [sbp(pe_, "WU%d" % i, [128, 8, 256], BF16) for i in range(NW)]
            WD = [sbp(pe_, "WD%d" % i, [128, 2, D], BF16) for i in range(NW)]
            WI = [sbp(pe_, "WI%d" % i, [128, 1], I32) for i in range(NW)]
            XE = [sbp(pe_, "XE%d" % i, [128, D], BF16) for i in range(4)]
            XT = [sbp(pe_, "XT%d" % i, [128, 8, TS], BF16) for i in range(2)]
            YE = [sbp(pe_, "YE%d" % i, [128, D], BF16) for i in range(4)]
            aT2 = [sbp(pe_, "aT2_%d" % i, [128, 2, TS], BF16) for i in range(2)]
            slt = [sbp(pe_, "slt%d" % i, [128, TS], BF16) for i in range(2)]
            ye_toks = []
            evi = 0
            xei = 0
            yei = 0

            def load_w(j):
                wi_t, wi_b = WI[j % NW]
                S.op("dve", lambda e: e.tensor_copy(out=wi_t[:], in_=widx[:, j:j + 1]), reads=[widxb], writes=[wi_b])
                indirect(WG[j % NW][0][:, :, :].rearrange("p c f -> p (c f)"), wbf[0][:, :], wi_t[:, :], False, [wi_b], [WG[j % NW][1]], bw_reg)
                indirect(WU[j % NW][0][:, :, :].rearrange("p c f -> p (c f)"), wbf[1][:, :], wi_t[:, :], False, [wi_b], [WU[j % NW][1]], bw_reg)
                indirect(WD[j % NW][0][:, :, :].rearrange("p c f -> p (c f)"), wbf[2][:, :], wi_t[:, :], False, [wi_b], [WD[j % NW][1]], bw_reg)

            for tok in conv_toks:
                S._wait("pool", tok)
            load_w(0)
            load_w(1)
            for tok in scat_toks:
                S._wait("sp", tok)

            def xload(j):
                for s in range(NSUB):
                    xe_t, xe_b = XE[(j % 2) * NSUB + s]
                    S.dma("sp", xe_t[:], xscr[j * TS + s * 128:j * TS + (s + 1) * 128, :], writes=[xe_b])

            def xpose(j):
                xT_t, xT_b = XT[j % 2]
                for s in range(NSUB):
                    xe_t, xe_b = XE[(j % 2) * NSUB + s]
                    pt, pb = PS[s % 2]
                    ptb = pt[:].bitcast(BF16)
                    for k in range(8):
                        S.op("pe", lambda e: e.transpose(ptb[:, k * 128:(k + 1) * 128], xe_t[:, k * 128:(k + 1) * 128], ident[:]), reads=[xe_b, identb], writes=[pb])
                    if s % 2 == 0:
                        S.op("act", lambda e: e.copy(out=xT_t[:, :, s * 128:(s + 1) * 128], in_=ptb.rearrange("p (c t) -> p c t", c=8)), reads=[pb], writes=[xT_b])
                    else:
                        S.op("dve", lambda e: e.tensor_copy(out=xT_t[:, :, s * 128:(s + 1) * 128], in_=ptb.rearrange("p (c t) -> p c t", c=8)), reads=[pb], writes=[xT_b])

            xload(0)
            xpose(0)
            for j in range(NT):
                if j + 2 < NT:
                    load_w(j + 2)
                if j + 1 < NT:
                    xload(j + 1)
                wg_t, wg_b = WG[j % NW]
                wu_t, wu_b = WU[j % NW]
                wd_t, wd_b = WD[j % NW]
                xT_t, xT_b = XT[j % 2]
                a_t, a_b = aT2[j % 2]
                for f in range(2):
                    pg, pgb2 = PS[2 + 2 * f]
                    pu, pub2 = PS[3 + 2 * f]
                    for k in range(8):
                        mm(pg[:, 0:TS], wg_t[:, k, f * 128:(f + 1) * 128], xT_t[:, k, :], k == 0, k == 7, [wg_b, xT_b], pgb2)
                    for k in range(8):
                        mm(pu[:, 0:TS], wu_t[:, k, f * 128:(f + 1) * 128], xT_t[:, k, :], k == 0, k == 7, [wu_b, xT_b], pub2)
                    sl_t, sl_b = slt[f]
                    S.op("act", lambda e: e.activation(out=sl_t[:], in_=pg[:, 0:TS], func=AF.Silu), reads=[pgb2], writes=[sl_b])
                    S.op("dve", lambda e: e.tensor_tensor(out=a_t[:, f, :], in0=pu[:, 0:TS], in1=sl_t[:], op=ALU.mult), reads=[pub2, sl_b], writes=[a_b])
                if j + 1 < NT:
                    xpose(j + 1)
                for s in range(NSUB):
                    ye_t, ye_b = YE[yei % 4]
                    yei += 1
                    for hf in range(2):
                        pd, pdb = PS[6 + evi % 2]
                        for f in range(2):
                            mm(pd[:], a_t[:, f, s * 128:(s + 1) * 128], wd_t[:, f, hf * 512:(hf + 1) * 512], f == 0, f == 1, [a_b, wd_b], pdb)
                        if evi % 2 == 0:
                            S.op("act", lambda e: e.copy(out=ye_t[:, hf * 512:(hf + 1) * 512], in_=pd[:]), reads=[pdb], writes=[ye_b])
                        else:
                            S.op("dve", lambda e: e.tensor_copy(out=ye_t[:, hf * 512:(hf + 1) * 512], in_=pd[:]), reads=[pdb], writes=[ye_b])
                        evi += 1
                    ye_toks.append(S.dma("sp", yscr[j * TS + s * 128:j * TS + (s + 1) * 128, :], ye_t[:], reads=[ye_b]))
            for tok in ye_toks:
                S._wait("pool", tok)
            S.barrier()
            GB = []
            for wl in (WG, WU):
                for i in range(NW):
                    flat_w = wl[i][0][:, :, :].rearrange("p c f -> p (c f)")
                    for hh in range(2):
                        GB.append((flat_w[:, hh * D:(hh + 1) * D], Buf("gbv%d" % len(GB))))
            NGB = 8
            gi_ = 0
            for i in range(16):
                for k2 in range(2):
                    g_t, g_b = GB[gi_ % NGB]
                    gi_ += 1
                    indirect(g_t, yscr[:, :], idxs[:, i, k2:k2 + 1], False, [idxsb], [g_b], bc_reg)
                    S.op("dve", lambda e: e.scalar_tensor_tensor(out=xres[:, i, :], in0=g_t, scalar=wts[:, i, k2:k2 + 1], in1=xres[:, i, :],
                                                                 op0=ALU.mult, op1=ALU.add), reads=[g_b, wtsb, xrb[i]], writes=[xrb[i]])
            S.barrier()
            pe_.close()
            stop_here("E", xres[:, 0:4, 0:512], xrb[0])
            pf = newstack()
            S.dma("sp", gb2[:], g_fin.partition_broadcast(128), writes=[gb2b])
            fo = [sbp(pf, "fo%d" % i, [128, D], F32) for i in range(4)]
            out_toks = []
            for i in range(16):
                f_t, f_b = fo[i % 4]
                s_t, s_b = st2[i % 4]
                S.op("act", lambda e: e.activation(out=sq2[:], in_=xres[:, i, :], func=AF.Square, accum_out=s_t[:, 0:1]), reads=[xrb[i]], writes=[sq2b, s_b])
                S.op("act", lambda e: e.activation(out=s_t[:, 1:2], in_=s_t[:, 0:1], func=AF.Sqrt, bias=eps_c[:], scale=1.0 / D), reads=[s_b, eps_cb], writes=[s_b])
                S.op("dve", lambda e: e.reciprocal(out=s_t[:, 2:3], in_=s_t[:, 1:2]), reads=[s_b], writes=[s_b])
                S.op("dve", lambda e: e.scalar_tensor_tensor(out=f_t[:], in0=xres[:, i, :], scalar=s_t[:, 2:3], in1=gb2[:], op0=ALU.mult, op1=ALU.mult),
                     reads=[xrb[i], s_b, gb2b], writes=[f_b])
                out_toks.append(S.dma("sp", out[i * 128:(i + 1) * 128, :], f_t[:], reads=[f_b]))
            for tok in out_toks:
                S._wait("sp", tok)
            S.barrier()
            for s_ in reversed(STACKS):
                s_.close()

        except StopBuildOuter:
            pass
    return nc


def kernel(**inputs):
    debug = inputs.pop("_debug", None)
    x = np.ascontiguousarray(inputs["x"], dtype=np.float32)
    pos = np.ascontiguousarray(inputs["positions"], dtype=np.int32)
    memv = np.ascontiguousarray(inputs["mem"], dtype=np.float32)
    nc = build_program(debug)
    shared = {}
    for k, v in inputs.items():
        if k in ("x", "mem", "positions"):
            continue
        a = np.ascontiguousarray(v)
        if k == "norm_final_g":
            shared[k] = a.reshape(1024)
        elif k == "moe_w_expert":
            shared[k] = a[0].reshape(1024, 32)
        elif k == "moe_b_expert":
            shared[k] = a[0].reshape(32)
        elif k in ("moe_w_gate", "moe_w_up"):
            shared["wg_l" if k == "moe_w_gate" else "wu_l"] = np.ascontiguousarray(a[0].reshape(32, 8, 128, 256).transpose(0, 2, 1, 3)).reshape(4096, 2048)
        elif k == "moe_w_down":
            shared["wd_l"] = np.ascontiguousarray(a[0].reshape(32, 2, 128, 1024).transpose(0, 2, 1, 3)).reshape(4096, 2048)
        else:
            shared[k] = a[0]
    in_maps = []
    for c in range(8):
        b, half = c // 2, c % 2
        xa = np.zeros((T_ALL, D), np.float32)
        pa = np.zeros((T_ALL,), np.int32)
        vm = np.ones((T_ALL,), np.float32)
        if half == 0:
            xa[T_OWN:] = x[b, :T_OWN]
            pa[T_OWN:] = pos[b, :T_OWN]
            vm[:T_OWN] = 0.0
        else:
            xa[:] = x[b]
            pa[:] = pos[b]
        m = dict(shared)
        m.update({"xa": xa, "posa": pa, "vmask": vm, "mem": memv[b]})
        in_maps.append(m)
    import os as _os
    _n = int(_os.environ.get('KCORES', '8'))
    res = run_bass_kernel_spmd(nc, in_maps[:_n], core_ids=list(range(_n)))
    if debug is not None:
        return [r["dbg"] for r in res.results]
    outp = np.zeros((4, 4096, D), np.float32)
    for c in range(8):
        b, half = c // 2, c % 2
        outp[b, half * T_OWN:(half + 1) * T_OWN] = res.results[c]["out"]
    return outp
```
